# Optimizing a Trainium2 kernel written in Bass

```python
import jax, jax.numpy as jnp
from jax import lax
import numpy as np

D_MODEL = 1024
BATCH = 8
SEQ = 2048
DEPTH = 2

GRID_W = 64
Q_BLOCK = 128
HEAD_DIM = 64
ROPE_THETA = 10000.0
RMS_EPS = 1e-6
LN_EPS = 1e-5
A_HEADS = 4
A_KV_HEADS = 2
B_HEADS = 4
NA_WIN_H = 8
NA_WIN_W = 16
C_HEADS = 4
C_NOPE = 64
C_ROPE = 32
C_V = 64
C_Q_RANK = 192
C_KV_RANK = 128
D_CH = 256
CONV_W = 31
N_BRANCH = 4
MIX_W = 256
D_FF = 2816
N_EXPERTS = 8
TOP_K = 2
D_FF_EXPERT = 3584
ALPHA = (2 * DEPTH) ** 0.25
BETA = (8 * DEPTH) ** -0.25
D_IN = ((A_HEADS + 2 * A_KV_HEADS) * HEAD_DIM + 3 * B_HEADS * HEAD_DIM
        + C_Q_RANK + C_KV_RANK + C_ROPE + 2 * D_CH + N_BRANCH * D_MODEL)

kernel_name = 'hybrid_gated_encoder_block'


def _split_points():
    sizes = (A_HEADS * HEAD_DIM, A_KV_HEADS * HEAD_DIM, A_KV_HEADS * HEAD_DIM,
             B_HEADS * HEAD_DIM, B_HEADS * HEAD_DIM, B_HEADS * HEAD_DIM,
             C_Q_RANK, C_KV_RANK, C_ROPE, 2 * D_CH, N_BRANCH * D_MODEL)
    return [int(v) for v in np.cumsum(sizes)[:-1]]


def layer_norm(x, g, b):
    xf = x.astype(jnp.float32)
    mu = jnp.mean(xf, axis=-1, keepdims=True)
    var = jnp.mean(jnp.square(xf - mu), axis=-1, keepdims=True)
    return ((xf - mu) * lax.rsqrt(var + LN_EPS) * g.astype(jnp.float32) + b.astype(jnp.float32)).astype(x.dtype)


def rms_norm(x, g):
    xf = x.astype(jnp.float32)
    return (xf * lax.rsqrt(jnp.mean(xf * xf, axis=-1, keepdims=True) + RMS_EPS) * g.astype(jnp.float32)).astype(x.dtype)


def axial_rope(seq, rot_dim):
    t = jnp.arange(seq)
    row = (t // GRID_W).astype(jnp.float32)
    col = (t % GRID_W).astype(jnp.float32)
    n_freq = rot_dim // 4
    inv = ROPE_THETA ** (-jnp.arange(n_freq, dtype=jnp.float32) / n_freq)
    ang = jnp.concatenate([row[:, None] * inv, col[:, None] * inv], axis=-1)
    return jnp.cos(ang), jnp.sin(ang)


def apply_rope(x, cos, sin):
    extra = x.ndim - 3
    c = cos.reshape(cos.shape[:1] + (1,) * extra + cos.shape[1:])
    s = sin.reshape(sin.shape[:1] + (1,) * extra + sin.shape[1:])
    xf = x.astype(jnp.float32)
    x1, x2 = xf[..., 0::2], xf[..., 1::2]
    out = jnp.stack([x1 * c - x2 * s, x1 * s + x2 * c], axis=-1).reshape(x.shape)
    return out.astype(x.dtype)


def block_attention(q, k, v):
    b, s, kvh, g, dk = q.shape
    nblk = s // Q_BLOCK
    scale = dk ** -0.5
    qb = q.reshape(b, nblk, Q_BLOCK, kvh, g, dk).transpose(1, 0, 2, 3, 4, 5)

    def one_block(qi):
        sc = jnp.einsum('bqhgd,bkhd->bhgqk', qi, k, preferred_element_type=jnp.float32) * scale
        p = jax.nn.softmax(sc, axis=-1)
        return jnp.einsum('bhgqk,bkhd->bqhgd', p.astype(v.dtype), v)

    out = lax.map(one_block, qb)
    return out.transpose(1, 0, 2, 3, 4, 5).reshape(b, s, kvh, g, v.shape[-1])


def neighbourhood_attention(q, k, v, rpb):
    b, s, h, d = q.shape
    rows = s // GRID_W
    wh = min(NA_WIN_H, rows)
    r = jnp.arange(rows)
    c = jnp.arange(GRID_W)
    row_start = jnp.clip(r - wh // 2, 0, rows - wh)
    key_rows = row_start[:, None] + jnp.arange(wh)[None, :]
    col_start = jnp.clip(c - NA_WIN_W // 2, 0, GRID_W - NA_WIN_W)
    col_ok = (c[None, :] >= col_start[:, None]) & (c[None, :] < col_start[:, None] + NA_WIN_W)
    qg = q.reshape(b, rows, GRID_W, h, d)
    kg = k.reshape(b, rows, GRID_W, h, d)[:, key_rows]
    vg = v.reshape(b, rows, GRID_W, h, d)[:, key_rows]
    sc = jnp.einsum('brqhd,brwkhd->bhrqwk', qg, kg, preferred_element_type=jnp.float32) * (d ** -0.5)
    dr_idx = key_rows - r[:, None] + (NA_WIN_H - 1)
    dc_idx = jnp.clip(c[None, :] - c[:, None] + (NA_WIN_W - 1), 0, 2 * NA_WIN_W - 2)
    bias = rpb[:, dr_idx[:, None, :, None], dc_idx[None, :, None, :]]
    sc = sc + bias.astype(jnp.float32)[None]
    sc = jnp.where(col_ok[:, None, :], sc, -jnp.inf)
    p = jax.nn.softmax(sc, axis=(-2, -1))
    out = jnp.einsum('bhrqwk,brwkhd->brqhd', p.astype(v.dtype), vg)
    return out.reshape(b, s, h * d)


def hybrid_mixer(x, w_in, a_q_norm, a_k_norm, b_rpb, c_q_norm, c_kv_norm, c_w_uq, c_w_ukv,
                 d_conv_w, d_conv_b, d_ln_g, d_ln_b, w_branch, w_out, rope_a, rope_c):
    b, s, _ = x.shape
    h = x @ w_in
    (a_q, a_k, a_v, b_q, b_k, b_v, c_cq, c_ckv, c_kpe, d_glu, gate_logits) = jnp.split(h, _split_points(), axis=-1)

    qa = apply_rope(rms_norm(a_q.reshape(b, s, A_HEADS, HEAD_DIM), a_q_norm), *rope_a)
    ka = apply_rope(rms_norm(a_k.reshape(b, s, A_KV_HEADS, HEAD_DIM), a_k_norm), *rope_a)
    va = a_v.reshape(b, s, A_KV_HEADS, HEAD_DIM)
    qa = qa.reshape(b, s, A_KV_HEADS, A_HEADS // A_KV_HEADS, HEAD_DIM)
    y_a = block_attention(qa, ka, va).reshape(b, s, MIX_W)

    y_b = neighbourhood_attention(b_q.reshape(b, s, B_HEADS, HEAD_DIM),
                                  b_k.reshape(b, s, B_HEADS, HEAD_DIM),
                                  b_v.reshape(b, s, B_HEADS, HEAD_DIM), b_rpb)

    q_c = (rms_norm(c_cq, c_q_norm) @ c_w_uq).reshape(b, s, C_HEADS, C_NOPE + C_ROPE)
    q_nope = q_c[..., :C_NOPE]
    q_pe = apply_rope(q_c[..., C_NOPE:], *rope_c)
    kv_c = (rms_norm(c_ckv, c_kv_norm) @ c_w_ukv).reshape(b, s, C_HEADS, C_NOPE + C_V)
    k_nope = kv_c[..., :C_NOPE]
    v_c = kv_c[..., C_NOPE:]
    k_pe = apply_rope(c_kpe, *rope_c)
    q_full = jnp.concatenate([q_nope, q_pe], axis=-1)[:, :, :, None, :]
    k_full = jnp.concatenate([k_nope, jnp.broadcast_to(k_pe[:, :, None, :], (b, s, C_HEADS, C_ROPE))], axis=-1)
    y_c = block_attention(q_full, k_full, v_c).reshape(b, s, MIX_W)

    u = d_glu[..., :D_CH] * jax.nn.sigmoid(d_glu[..., D_CH:])
    u = lax.conv_general_dilated(u, d_conv_w, window_strides=(1,), padding=[(CONV_W // 2, CONV_W // 2)],
                                 dimension_numbers=('NWC', 'WIO', 'NWC'), feature_group_count=D_CH) + d_conv_b
    y_d = jax.nn.silu(layer_norm(u, d_ln_g, d_ln_b))

    ys = jnp.stack([y_a, y_b, y_c, y_d], axis=2)
    branches = jnp.einsum('bsnc,ncd->bsnd', ys, w_branch)
    gates = jax.nn.sigmoid(gate_logits.reshape(b, s, N_BRANCH, D_MODEL))
    merged = jnp.sum(gates * branches, axis=2)
    return merged @ w_out


def swiglu(x, w1, w3, w2):
    return (jax.nn.silu(x @ w1) * (x @ w3)) @ w2


def moe_swiglu(x, router, w1, w3, w2):
    logits = jnp.einsum('bsd,de->bse', x.astype(jnp.float32), router.astype(jnp.float32))
    top_logit, top_idx = lax.top_k(logits, TOP_K)
    top_w = jax.nn.softmax(top_logit, axis=-1)
    gate = jnp.sum(jax.nn.one_hot(top_idx, N_EXPERTS, dtype=jnp.float32) * top_w[..., None], axis=-2)
    gate = gate.astype(x.dtype)
    out = jnp.zeros_like(x)
    for e in range(N_EXPERTS):
        out = out + gate[..., e:e + 1] * swiglu(x, w1[e], w3[e], w2[e])
    return out


def setup_inputs(seed: int = 0) -> dict:
    key = jax.random.key(seed)
    ks = jax.random.split(key, 28)

    def nrm(k, shape, scale):
        return jax.random.normal(k, shape, jnp.float32) * scale

    def gain(k, shape):
        return 1.0 + 0.05 * jax.random.normal(k, shape, jnp.float32)

    n_dense = (DEPTH + 1) // 2
    n_moe = DEPTH // 2
    return {
        'x': nrm(ks[0], (BATCH, SEQ, D_MODEL), 1.0),
        'ln_in_g': gain(ks[1], (D_MODEL,)),
        'ln_in_b': nrm(ks[2], (D_MODEL,), 0.02),
        'w_in': nrm(ks[3], (DEPTH, D_MODEL, D_IN), D_MODEL ** -0.5),
        'a_q_norm': gain(ks[4], (DEPTH, HEAD_DIM)),
        'a_k_norm': gain(ks[5], (DEPTH, HEAD_DIM)),
        'b_rpb': nrm(ks[6], (DEPTH, B_HEADS, 2 * NA_WIN_H - 1, 2 * NA_WIN_W - 1), 0.1),
        'c_q_norm': gain(ks[7], (DEPTH, C_Q_RANK)),
        'c_kv_norm': gain(ks[8], (DEPTH, C_KV_RANK)),
        'c_w_uq': nrm(ks[9], (DEPTH, C_Q_RANK, C_HEADS * (C_NOPE + C_ROPE)), C_Q_RANK ** -0.5),
        'c_w_ukv': nrm(ks[10], (DEPTH, C_KV_RANK, C_HEADS * (C_NOPE + C_V)), C_KV_RANK ** -0.5),
        'd_conv_w': nrm(ks[11], (DEPTH, CONV_W, 1, D_CH), CONV_W ** -0.5),
        'd_conv_b': nrm(ks[12], (DEPTH, D_CH), 0.02),
        'd_ln_g': gain(ks[13], (DEPTH, D_CH)),
        'd_ln_b': nrm(ks[14], (DEPTH, D_CH), 0.02),
        'w_branch': nrm(ks[15], (DEPTH, N_BRANCH, MIX_W, D_MODEL), MIX_W ** -0.5),
        'w_out': nrm(ks[16], (DEPTH, D_MODEL, D_MODEL), BETA * D_MODEL ** -0.5),
        'ln_mix_g': gain(ks[17], (DEPTH, D_MODEL)),
        'ln_mix_b': nrm(ks[18], (DEPTH, D_MODEL), 0.02),
        'ffn_w1': nrm(ks[19], (n_dense, D_MODEL, D_FF), D_MODEL ** -0.5),
        'ffn_w3': nrm(ks[20], (n_dense, D_MODEL, D_FF), D_MODEL ** -0.5),
        'ffn_w2': nrm(ks[21], (n_dense, D_FF, D_MODEL), BETA * D_FF ** -0.5),
        'moe_router': nrm(ks[22], (n_moe, D_MODEL, N_EXPERTS), D_MODEL ** -0.5),
        'moe_w1': nrm(ks[23], (n_moe, N_EXPERTS, D_MODEL, D_FF_EXPERT), D_MODEL ** -0.5),
        'moe_w3': nrm(ks[24], (n_moe, N_EXPERTS, D_MODEL, D_FF_EXPERT), D_MODEL ** -0.5),
        'moe_w2': nrm(ks[25], (n_moe, N_EXPERTS, D_FF_EXPERT, D_MODEL), BETA * D_FF_EXPERT ** -0.5),
        'ln_ffn_g': gain(ks[26], (DEPTH, D_MODEL)),
        'ln_ffn_b': nrm(ks[27], (DEPTH, D_MODEL), 0.02),
    }


def reference(x, ln_in_g, ln_in_b, w_in, a_q_norm, a_k_norm, b_rpb, c_q_norm, c_kv_norm, c_w_uq, c_w_ukv,
              d_conv_w, d_conv_b, d_ln_g, d_ln_b, w_branch, w_out, ln_mix_g, ln_mix_b,
              ffn_w1, ffn_w3, ffn_w2, moe_router, moe_w1, moe_w3, moe_w2, ln_ffn_g, ln_ffn_b):
    s = x.shape[1]
    rope_a = axial_rope(s, HEAD_DIM)
    rope_c = axial_rope(s, C_ROPE)
    x = layer_norm(x, ln_in_g, ln_in_b)
    for l in range(DEPTH):
        mix = hybrid_mixer(x, w_in[l], a_q_norm[l], a_k_norm[l], b_rpb[l], c_q_norm[l], c_kv_norm[l],
                           c_w_uq[l], c_w_ukv[l], d_conv_w[l], d_conv_b[l], d_ln_g[l], d_ln_b[l],
                           w_branch[l], w_out[l], rope_a, rope_c)
        x = layer_norm(ALPHA * x + mix, ln_mix_g[l], ln_mix_b[l])
        if l % 2 == 0:
            f = swiglu(x, ffn_w1[l // 2], ffn_w3[l // 2], ffn_w2[l // 2])
        else:
            f = moe_swiglu(x, moe_router[l // 2], moe_w1[l // 2], moe_w3[l // 2], moe_w2[l // 2])
        x = layer_norm(ALPHA * x + f, ln_ffn_g[l], ln_ffn_b[l])
    return x
```

```python
import numpy as np
import concourse.bass as bass
import concourse.mybir as mybir
from concourse.bass_utils import run_bass_kernel_spmd

F32 = mybir.dt.float32
BF16 = mybir.dt.bfloat16
AF = mybir.ActivationFunctionType
ALU = mybir.AluOpType

D = 1024
S = 2048
NT = 16
NG = 4
KC = 8
DEPTH = 2
GRID_W = 64
D_IN = 6240
D_FF = 2816
NE = 8
D_FFE = 3584
ALPHA = (2 * DEPTH) ** 0.25
RMS_EPS = 1e-6
LN_EPS = 1e-5
NEG = -30000.0

ENGS = ("pe", "act", "dve", "pool", "sp")


class Buf:
    __slots__ = ("name", "writer", "readers", "dsem", "dcount", "excl")

    def __init__(self, name, excl=False):
        self.excl = excl
        self.name = name
        self.writer = None
        self.readers = {}
        self.dsem = None
        self.dcount = 0


class Ins:
    __slots__ = ("eng", "fn", "deps", "signal", "sem", "val", "is_dma")

    def __init__(self, eng, fn):
        self.eng = eng
        self.fn = fn
        self.deps = []
        self.signal = False
        self.sem = None
        self.val = None
        self.is_dma = False


class Sched:
    def __init__(self, nc):
        self.nc = nc
        self.q = {e: [] for e in ENGS}
        self.esem = {}
        self.shared_dsem = None

    def _add(self, eng, fn, reads, writes, is_dma=False, dma_buf=None):
        ins = Ins(eng, fn)
        ins.is_dma = is_dma
        writes = writes + [r for r in reads if r.excl and r not in writes]
        deps = []
        for r in reads:
            if r.writer is not None:
                deps.append(r.writer)
        for w in writes:
            if w.writer is not None:
                deps.append(w.writer)
            deps.extend(w.readers.values())
        seen = set()
        for d in deps:
            if d is ins or id(d) in seen:
                continue
            seen.add(id(d))
            if (not d.is_dma) and (not is_dma) and d.eng == "pe" and eng == "pe":
                continue
            ins.deps.append(d)
            d.signal = True
        if is_dma:
            b = dma_buf
            if b.dsem is None:
                b.dsem = self.nc.alloc_semaphore("d_" + b.name)
            b.dcount += 16
            ins.sem = b.dsem
            ins.val = b.dcount
            ins.signal = True
        for w in writes:
            w.writer = ins
            w.readers = {}
        for r in reads:
            if r in writes:
                continue
            r.readers[("dma", id(ins)) if is_dma else eng] = ins
        self.q[eng].append(ins)
        return ins

    def op(self, eng, fn, reads=(), writes=()):
        return self._add(eng, fn, list(reads), list(writes))

    def dma(self, eng, out, in_, reads=(), writes=(), track=None):
        reads = list(reads)
        writes = list(writes)
        b = track if track is not None else (writes[0] if writes else reads[0])

        def fn(e, out=out, in_=in_):
            return e.dma_start(out=out, in_=in_)
        return self._add(eng, fn, reads, writes, is_dma=True, dma_buf=b)

    def emit(self, final_waits=()):
        nc = self.nc
        for e in ENGS:
            self.esem[e] = nc.alloc_semaphore("e_" + e)
        for e in ENGS:
            t = 0
            for ins in self.q[e]:
                if ins.is_dma:
                    continue
                if ins.signal:
                    t += 1
                    ins.sem = self.esem[e]
                    ins.val = t
        sched = self

        def run(engname, eng):
            waited = {}
            for ins in sched.q[engname]:
                for d in ins.deps:
                    key = id(d.sem)
                    if waited.get(key, 0) >= d.val:
                        continue
                    waited[key] = d.val
                    eng.wait_ge(d.sem, d.val)
                bi = ins.fn(eng)
                if ins.is_dma:
                    bi.then_inc(ins.sem, 16)
                elif ins.signal:
                    bi.then_inc(ins.sem, 1)
            if engname == "sp":
                for b in final_waits:
                    if b.dsem is not None and b.dcount > 0:
                        eng.wait_ge(b.dsem, b.dcount)

        with nc.Block() as block:
            @block.tensor
            def _(e):
                run("pe", e)

            @block.scalar
            def _(e):
                run("act", e)

            @block.vector
            def _(e):
                run("dve", e)

            @block.gpsimd
            def _(e):
                run("pool", e)

            @block.sync
            def _(e):
                run("sp", e)


C_ID, C_ONES, C_BD, C_RM, C_MASK, C_W = 0, 128, 256, 384, 512, 576


def make_consts():
    c = np.zeros((128, C_W), np.float32)
    c[:, C_ID:C_ID + 128] = np.eye(128, dtype=np.float32)
    c[:, C_ONES:C_ONES + 128] = 1.0
    bd = np.zeros((128, 128), np.float32)
    bd[:64, :64] = 1.0
    bd[64:, 64:] = 1.0
    c[:, C_BD:C_BD + 128] = bd
    rm = np.zeros((128, 128), np.float32)
    for i in range(64):
        rm[2 * i + 1, 2 * i] = -1.0
        rm[2 * i, 2 * i + 1] = 1.0
    c[:, C_RM:C_RM + 128] = rm
    cc = np.arange(GRID_W)
    col_start = np.clip(cc - 8, 0, GRID_W - 16)
    ok = (cc[None, :] >= col_start[:, None]) & (cc[None, :] < col_start[:, None] + 16)
    m = np.where(ok.T, 0.0, NEG).astype(np.float32)
    c[:64, C_MASK:C_MASK + 64] = m
    c[64:, C_MASK:C_MASK + 64] = m
    return c


def make_rope(rot_dim, reps):
    t = np.arange(S)
    row = (t // GRID_W).astype(np.float32)
    col = (t % GRID_W).astype(np.float32)
    n_freq = rot_dim // 4
    inv = (10000.0 ** (-np.arange(n_freq, dtype=np.float32) / n_freq)).astype(np.float32)
    ang = np.concatenate([row[:, None] * inv, col[:, None] * inv], axis=-1)
    cos = np.cos(ang).astype(np.float32)
    sin = np.sin(ang).astype(np.float32)
    cosT = np.repeat(cos.T, 2, axis=0)
    sinT = np.repeat(sin.T, 2, axis=0)
    out = np.stack([np.tile(cosT, (reps, 1)), np.tile(sinT, (reps, 1))], axis=1)
    return np.ascontiguousarray(out.astype(np.float32))


class MK:
    def __init__(self, upto="all", debug=(), light=False, skip=""):
        self.light = light
        self.skip = skip
        self.upto = upto
        self.debug = set(debug)
        self.nc = bass.Bass("TRN2", target_bir_lowering=False)
        self.S = Sched(self.nc)
        self.dbg_out = {}
        self._uid = 0
        self.phase_bufs = []
        self.cur_barrier = None
        self.build()

    def uid(self, p):
        self._uid += 1
        return f"{p}{self._uid}"

    def at(self, name, shape, dtype, off):
        return self.nc.alloc_sbuf_tensor_at(self.uid(name), list(shape), dtype, offset=off)

    def din(self, name, shape, dtype=F32):
        return self.nc.dram_tensor(name, list(shape), dtype, kind="ExternalInput").ap()

    def nb(self, name):
        b = Buf(self.uid(name))
        b.writer = self.cur_barrier
        self.phase_bufs.append(b)
        return b

    def barrier(self):
        bufs = self.phase_bufs
        junk = self.junk
        ins = self.S.op("dve", lambda e: e.memset(junk[:, 0:1], 0.0), reads=[], writes=bufs + [self.bjunk])
        self.cur_barrier = ins
        self.phase_bufs = []

    class Ring:
        def __init__(self, items):
            self.items = items
            self.i = 0

        def get(self):
            it = self.items[self.i % len(self.items)]
            self.i += 1
            return it

    def ring_at(self, name, shape, dtype, off, n, nbytes, phase=True):
        return MK.Ring([(self.at(name, shape, dtype, off + i * nbytes), self.nb(f"{name}{i}") if phase else Buf(f"{name}{i}"))
                        for i in range(n)])

    def _spacer(self, is32):
        if (not is32) and getattr(self, "last_fp32", False) and self.use_spacer:
            spb, idb = self.spb, self.idb
            self.S.op("pe", lambda e: e.matmul(spb[0:32, 0:32], lhsT=idb[:, 0:32], rhs=idb[:, 0:32], start=True, stop=True),
                      reads=[self.bcst], writes=[self.bspb])
        self.last_fp32 = is32

    def mm(self, out, ob, lhsT, rhs, rb, start=True, stop=True):
        self._spacer(lhsT.dtype == F32)
        self.S.op("pe", lambda e: e.matmul(out, lhsT=lhsT, rhs=rhs, start=start, stop=stop), reads=rb, writes=[ob])

    def tr(self, out, ob, in_, ident, rb):
        self._spacer(in_.dtype == F32)
        self.S.op("pe", lambda e: e.transpose(out, in_, ident), reads=rb, writes=[ob])

    def act(self, out, ob, in_, rb, func, scale=1.0, bias=0.0):
        self.S.op("act", lambda e: e.activation(out=out, in_=in_, func=func, bias=bias, scale=scale),
                  reads=rb, writes=[ob])

    def tt(self, out, ob, in0, in1, rb, op, eng="dve"):
        self.S.op(eng, lambda e: e.tensor_tensor(out=out, in0=in0, in1=in1, op=op), reads=rb, writes=[ob])

    def ts(self, out, ob, in0, s1, s2, rb, op0, op1=None, eng="dve"):
        if op1 is None:
            self.S.op(eng, lambda e: e.tensor_scalar(out=out, in0=in0, scalar1=s1, scalar2=None, op0=op0),
                      reads=rb, writes=[ob])
        else:
            self.S.op(eng, lambda e: e.tensor_scalar(out=out, in0=in0, scalar1=s1, scalar2=s2, op0=op0, op1=op1),
                      reads=rb, writes=[ob])

    def stt(self, out, ob, in0, sc, in1, rb, op0, op1):
        self.S.op("dve", lambda e: e.scalar_tensor_tensor(out=out, in0=in0, scalar=sc, in1=in1, op0=op0, op1=op1),
                  reads=rb, writes=[ob])

    def cp(self, out, ob, in_, rb, eng="dve", scale=None):
        if eng == "act":
            sc = 1.0 if scale is None else scale
            self.S.op("act", lambda e: e.activation(out=out, in_=in_, func=AF.Copy, scale=sc), reads=rb, writes=[ob])
        else:
            self.S.op(eng, lambda e: e.tensor_copy(out=out, in_=in_), reads=rb, writes=[ob])

    def dump(self, key, ap, buf, shape, dtype=F32):
        if key not in self.debug:
            return
        d = self.nc.dram_tensor("dbg_" + key, list(shape), dtype, kind="ExternalOutput").ap()
        b = Buf("dbg_" + key)
        self.S.dma("sp", d, ap, reads=[buf], track=b)
        self.dbg_out[key] = b

    def wload(self, src_rows, nk, ncols):
        t, b = self.wring.get()
        v = t[:, 0:nk * ncols].rearrange("p (k c) -> p k c", k=nk)
        self.S.dma("pool", v, src_rows.rearrange("(k p) c -> p k c", p=128), writes=[b])
        return v, b

    def wload_segs(self, src2d, nk, segs):
        t, b = self.wring.get()
        tot = sum(n for _, n in segs)
        v = t[:, 0:nk * tot].rearrange("p (k c) -> p k c", k=nk)
        o = 0
        for (c0, n) in segs:
            self.S.dma("pool", v[:, :, o:o + n], src2d[:, c0:c0 + n].rearrange("(k p) c -> p k c", p=128), writes=[b])
            o += n
        return v, b

    def psum(self):
        return self.pring.get()

    def tmp(self):
        return self.tring.get()

    def build(self):
        nc, S_ = self.nc, self.S
        I = {}
        I["x"] = self.din("x", [S, D])
        I["cst"] = self.din("cst", [128, C_W])
        I["ropeA"] = self.din("ropeA", [128, 2, S])
        I["ropeC"] = self.din("ropeC", [32, 2, S])
        I["ln_in_g"] = self.din("ln_in_g", [D])
        I["ln_in_b"] = self.din("ln_in_b", [D])
        I["w_in"] = self.din("w_in", [DEPTH, D, D_IN])
        I["a_q_norm"] = self.din("a_q_norm", [DEPTH, 64])
        I["a_k_norm"] = self.din("a_k_norm", [DEPTH, 64])
        I["rpbT"] = self.din("rpbT", [DEPTH, 4, 15 * 64, 64])
        I["c_q_norm"] = self.din("c_q_norm", [DEPTH, 192])
        I["c_kv_norm"] = self.din("c_kv_norm", [DEPTH, 128])
        I["c_w_uq"] = self.din("c_w_uq", [DEPTH, 192, 384])
        I["c_w_ukv"] = self.din("c_w_ukv", [DEPTH, 128, 512])
        I["d_conv_w"] = self.din("d_conv_w", [DEPTH, 31, 256])
        I["d_conv_b"] = self.din("d_conv_b", [DEPTH, 256])
        I["d_ln_g"] = self.din("d_ln_g", [DEPTH, 256])
        I["d_ln_b"] = self.din("d_ln_b", [DEPTH, 256])
        I["w_branch"] = self.din("w_branch", [DEPTH, 4, 256, D])
        I["w_out"] = self.din("w_out", [DEPTH, D, D])
        I["ln_mix_g"] = self.din("ln_mix_g", [DEPTH, D])
        I["ln_mix_b"] = self.din("ln_mix_b", [DEPTH, D])
        if not self.light:
          I["ffn_w1"] = self.din("ffn_w1", [1, D, D_FF])
          I["ffn_w3"] = self.din("ffn_w3", [1, D, D_FF])
          I["ffn_w2"] = self.din("ffn_w2", [1, D_FF, D])
          I["moe_router"] = self.din("moe_router", [1, D, NE])
          I["moe_w1"] = self.din("moe_w1", [1, NE, D, D_FFE])
          I["moe_w3"] = self.din("moe_w3", [1, NE, D, D_FFE])
          I["moe_w2"] = self.din("moe_w2", [1, NE, D_FFE, D])
        I["ln_ffn_g"] = self.din("ln_ffn_g", [DEPTH, D])
        I["ln_ffn_b"] = self.din("ln_ffn_b", [DEPTH, D])
        self.I = I
        self.out = nc.dram_tensor("out", [S, D], F32, kind="ExternalOutput").ap()
        self.out_buf = Buf("out")
        self.out_bufs = []

        o = 16384
        self.cst = self.at("cst", [128, C_W], F32, o); o += C_W * 4
        self.bcst = Buf("cst")
        self.vec = self.at("vec", [128, 128], F32, o); o += 512
        self.bvec = Buf("vec")
        self.idb = self.at("idb", [128, 128], BF16, o); o += 256
        self.XH = self.at("XH", [128, KC, S], BF16, o); o += KC * S * 2
        self.XL = self.at("XL", [128, KC, S], BF16, o); o += KC * S * 2
        self.bX = [Buf(f"X{g}") for g in range(NG)]
        self.WOFF = o
        self.wring = self.ring_at("ws", [128, 4096], BF16, o, 4, 8192, phase=False); o += 4 * 8192
        self.wring4 = self.wring
        ntmp = 8
        self.tring = self.ring_at("tmp", [128, 512], F32, o, ntmp, 2048, phase=False); o += ntmp * 2048
        self.sm = self.at("sm", [128, 120], F32, o); o += 480
        self.bsm = Buf("sm")
        self.lnm = self.at("lnm", [128, 512], F32, o); o += 2048
        self.lnv = self.at("lnv", [128, 512], F32, o); o += 2048
        self.blnm, self.blnv = Buf("lnm"), Buf("lnv")
        self.junk = self.at("junk", [128, 8], F32, o); o += 32
        self.bjunk = Buf("junk")
        self.SCR = o
        self.SCR_END = 229376
        banks = []
        for i in range(8):
            banks.append((nc.alloc_psum_tensor(f"ps{i}", [128, 512], F32), Buf(f"ps{i}", excl=True)))
        self.pring = MK.Ring(banks[0:6])
        self.aring = MK.Ring(banks[6:8])
        self.use_spacer = False

        self.ident = self.cst[:, C_ID:C_ID + 128]
        self.ones = self.cst[:, C_ONES:C_ONES + 128]
        self.bd = self.cst[:, C_BD:C_BD + 128]
        self.rm = self.cst[:, C_RM:C_RM + 128]
        self.maskB = self.cst[:, C_MASK:C_MASK + 64]

        S_.dma("sp", self.cst[:], I["cst"], writes=[self.bcst])
        self.cp(self.idb[:], self.bcst, self.ident, [self.bcst])

        self.PH = self.SCR + 32768
        self.barrier()
        self.phase0()
        if self.upto == "p0":
            return self.finish()
        for l in range(DEPTH):
            done = self.layer(l)
            if done:
                break
        return self.finish()

    def finish(self):
        fw = [self.out_buf] + self.out_bufs + list(self.dbg_out.values())
        self.S.emit(final_waits=fw)

    def hilo(self, src, sb, fcs, cols, g):
        hi = self.XH[:, fcs, cols]
        lo = self.XL[:, fcs, cols]
        self.act(hi, self.bX[g], src, [sb], AF.Copy)
        self.tt(lo, self.bX[g], src, hi, [sb, self.bX[g]], ALU.subtract)

    def phase0(self):
        I, S_ = self.I, self.S
        base = self.PH
        gb = self.at("gb", [128, 2, D], F32, base)
        bgb = self.nb("gb")
        S_.dma("sp", gb[:, 0, :], I["ln_in_g"].partition_broadcast(128), writes=[bgb])
        S_.dma("sp", gb[:, 1, :], I["ln_in_b"].partition_broadcast(128), writes=[bgb])
        xin = self.ring_at("xin", [128, D], F32, base + 8192, 2, 4096)
        xn = self.ring_at("xn", [128, D], F32, base + 16384, 2, 4096)
        for tt in range(NT):
            g = tt // 4
            xt, bx = xin.get()
            S_.dma("sp", xt[:], I["x"][tt * 128:(tt + 1) * 128, :], writes=[bx])
            st = self.sm[:, 0:12].rearrange("p (a b) -> p a b", a=2)
            mv = self.sm[:, 12:14]
            for c in range(2):
                S_.op("dve", lambda e, c=c, xt=xt, st=st: e.bn_stats(out=st[:, c, :], in_=xt[:, c * 512:(c + 1) * 512]),
                      reads=[bx], writes=[self.bsm])
            S_.op("dve", lambda e, st=st, mv=mv: e.bn_aggr(out=mv, in_=st), reads=[self.bsm], writes=[self.bsm])
            rs = self.sm[:, 14:15]
            self.act(rs, self.bsm, mv[:, 1:2], [self.bsm], AF.Ln, bias=LN_EPS)
            self.act(rs, self.bsm, rs, [self.bsm], AF.Exp, scale=-0.5)
            xo, bxo = xn.get()
            self.ts(xo[:], bxo, xt[:], mv[:, 0:1], rs, [bx, self.bsm], ALU.subtract, ALU.mult)
            self.tt(xo[:], bxo, xo[:], gb[:, 0, :], [bxo, bgb], ALU.mult)
            self.tt(xo[:], bxo, xo[:], gb[:, 1, :], [bxo, bgb], ALU.add)
            for half in range(2):
                pt, bp = self.psum()
                for j in range(4):
                    fc = half * 4 + j
                    self.tr(pt[:, j * 128:(j + 1) * 128], bp, xo[:, fc * 128:(fc + 1) * 128], self.ident, [bxo, self.bcst])
                self.hilo(pt[:].rearrange("p (a b) -> p a b", a=4), bp, slice(half * 4, half * 4 + 4),
                          slice(tt * 128, (tt + 1) * 128), g)
        self.dump("xh0", self.XH[:], self.bX[3], [128, KC, S], BF16)
        self.dump("xl0", self.XL[:], self.bX[3], [128, KC, S], BF16)

    def proj(self, w, wb, cols, g, m=128):
        pt, bp = self.psum()
        for kc in range(KC):
            self.mm(pt[0:m, :], bp, w[:, kc, cols], self.XH[:, kc, g * 512:(g + 1) * 512], [wb, self.bX[g]],
                    start=(kc == 0), stop=(kc == KC - 1))
        return pt, bp

    def rstd_from(self, ssp, bss, n, eps, m=128):
        r, br = self.tmp()
        self.act(r[0:m, :], br, ssp[0:m, :], [bss], AF.Ln, scale=1.0 / n, bias=eps)
        self.act(r[0:m, :], br, r[0:m, :], [br], AF.Exp, scale=-0.5)
        return r, br

    def rope(self, qn, bqn, m, cos, sin, brope, out, bout=None):
        dsts = out if isinstance(out, list) else [(out, bout, slice(0, m))]
        pr, bpr = self.psum()
        self.mm(pr[0:m, :], bpr, self.rm[0:m, 0:m], qn[0:m, :], [self.bcst, bqn])
        t1, bt1 = self.tmp()
        self.tt(t1[0:m, :], bt1, qn[0:m, :], cos, [bqn, brope], ALU.mult)
        t2, bt2 = self.tmp()
        self.tt(t2[0:m, :], bt2, pr[0:m, :], sin, [bpr, brope], ALU.mult)
        for (o_, b_, ps_) in dsts:
            self.tt(o_, b_, t1[ps_, :], t2[ps_, :], [bt1, bt2], ALU.add)

    def attention(self, nheads, qk_ops, v_ap, bv, scale, br_idx, PT, ytm_ring):
        steps = [(tq, h, kg) for tq in range(NT) for h in range(nheads) for kg in range(4)]

        def emit_qk(st):
            tq, h, kg = st
            ps_, bps = self.psum()
            for j in range(4):
                kt = kg * 4 + j
                ops, rb = qk_ops(h, tq, kt)
                for i, (lh, rh) in enumerate(ops):
                    self.mm(ps_[:, j * 128:(j + 1) * 128], bps, lh, rh, rb, start=(i == 0), stop=(i == len(ops) - 1))
            return ps_, bps

        cur = {}

        def emit_rest(st, ps_, bps):
            tq, h, kg = st
            if h == 0 and kg == 0:
                cur["ytm"], cur["bytm"] = ytm_ring.get()
                cur["po"], cur["bpo"] = self.aring.get()
            po, bpo = cur["po"], cur["bpo"]
            pt_, bpt = PT.get()
            self.act(pt_[:], bpt, ps_[:], [bps], AF.Exp, scale=scale)
            for j in range(4):
                kt = kg * 4 + j
                self.mm(po[:, h * 65:(h + 1) * 65], bpo, pt_[:, j * 128:(j + 1) * 128], v_ap(h, kt), [bpt, bv],
                        start=(kt == 0), stop=(kt == 15))
            if h == nheads - 1 and kg == 3:
                self.norm_out(po, bpo, nheads, cur["ytm"], cur["bytm"], br_idx, tq)

        LOOK = 2
        q = [emit_qk(steps[j]) for j in range(min(LOOK, len(steps)))]
        for i, st in enumerate(steps):
            if i + LOOK < len(steps):
                q.append(emit_qk(steps[i + LOOK]))
            emit_rest(st, *q.pop(0))

    def norm_out(self, po, bpo, nheads, ytm, bytm, br_idx, tq):
        pov = po[:, 0:nheads * 65].rearrange("p (h c) -> p h c", h=nheads)
        rinv = self.sm[:, 16:16 + nheads]
        self.S.op("dve", lambda e: e.reciprocal(out=rinv, in_=pov[:, :, 64]), reads=[bpo], writes=[self.bsm])
        for h in range(nheads):
            self.ts(ytm[:, h * 64:(h + 1) * 64], bytm, pov[:, h, 0:64], rinv[:, h:h + 1], None, [bpo, self.bsm], ALU.mult)
        pt, bp = self.psum()
        for j in range(2):
            self.tr(pt[:, j * 128:(j + 1) * 128], bp, ytm[:, j * 128:(j + 1) * 128], self.ident, [bytm, self.bcst])
        self.act(self.yT[br_idx][:, :, tq * 128:(tq + 1) * 128], self.byT[br_idx],
                 pt[:, 0:256].rearrange("p (a b) -> p a b", a=2), [bp], AF.Copy)

    def ln_fm(self, tT, bt, gc0, bc0, g, final=False):
        s1, bs1 = self.psum()
        for fc in range(KC):
            self.mm(s1[:], bs1, self.ones, tT[:, fc, :], [self.bcst, bt], start=(fc == 0), stop=(fc == KC - 1))
        s2, bs2 = self.psum()
        for fc in range(KC):
            sq, bsq = self.tmp()
            self.act(sq[:], bsq, tT[:, fc, :], [bt], AF.Square)
            self.mm(s2[:], bs2, self.ones, sq[:], [self.bcst, bsq], start=(fc == 0), stop=(fc == KC - 1))
        mean, bmean = self.lnm, self.blnm
        self.act(mean[:], bmean, s1[:], [bs1], AF.Copy, scale=1.0 / D)
        var, bvar = self.lnv, self.blnv
        self.act(var[:], bvar, s1[:], [bs1], AF.Square, scale=1.0 / D)
        self.stt(var[:], bvar, s2[:], 1.0 / D, var[:], [bs2, bvar], ALU.mult, ALU.subtract)
        self.act(var[:], bvar, var[:], [bvar], AF.Ln, bias=LN_EPS)
        self.act(var[:], bvar, var[:], [bvar], AF.Exp, scale=-0.5)
        self.stt(mean[:], bmean, mean[:], -1.0, var[:], [bmean, bvar], ALU.mult, ALU.mult)
        for fc in range(KC):
            u, bu = self.tmp()
            self.tt(u[:], bu, tT[:, fc, :], var[:], [bt, bvar], ALU.mult)
            self.tt(u[:], bu, u[:], mean[:], [bu, bmean], ALU.add)
            gcol, bcol = self.vec[:, gc0 + fc:gc0 + fc + 1], self.vec[:, bc0 + fc:bc0 + fc + 1]
            self.S.op("act", lambda e, u=u, gcol=gcol, bcol=bcol: e.activation(out=u[:], in_=u[:], func=AF.Identity, scale=gcol, bias=bcol),
                      reads=[bu, self.bvec], writes=[bu])
            if not final:
                self.hilo(u[:], bu, fc, slice(g * 512, (g + 1) * 512), g)
            else:
                pt, bp = self.psum()
                for t4 in range(4):
                    self.tr(pt[:, t4 * 128:(t4 + 1) * 128], bp, u[:, t4 * 128:(t4 + 1) * 128], self.ident, [bu, self.bcst])
                og, bog = self.oring.get()
                self.cp(og[:], bog, pt[:].rearrange("p (a b) -> p a b", a=4), [bp], eng="act")
                dst = self.out[g * 512:(g + 1) * 512, fc * 128:(fc + 1) * 128].rearrange("(a p) c -> p a c", p=128)
                self.S.dma("sp", dst, og[:], reads=[bog], track=bog)
                if bog not in self.out_bufs:
                    self.out_bufs.append(bog)

    def layer(self, l):
        if l == 0:
            self.yT = [self.at(f"yT{n}", [128, 2, S], BF16, self.SCR + n * 8192) for n in range(4)]
        self.load_vecs(l)
        steps = [("C", self.branch_C), ("A", self.branch_A), ("B", self.branch_B), ("D", self.branch_D),
                 ("M", self.merge), ("F", self.ffn)]
        self.byT = [Buf(f"yT{n}_{l}") for n in range(4)]
        for b in self.byT:
            b.writer = self.cur_barrier
        for name, fn in steps:
            if name == "F":
                self.phase_bufs.extend(self.byT)
            self.barrier()
            if name in self.skip:
                continue
            fn(l)
            if self.upto.startswith(f"L{l}{name}"):
                return True
        self.barrier()
        return False

    V_AQ, V_AK, V_CQ, V_CKV, V_DCB, V_DLG, V_DLB, V_LMG, V_LMB, V_LFG, V_LFB = 0, 1, 2, 4, 5, 7, 9, 11, 19, 27, 35

    def load_vecs(self, l):
        I, S_, v, b = self.I, self.S, self.vec, self.bvec

        def col(ap1d):
            return ap1d.rearrange("(p o) -> p o", o=1)
        for half in range(2):
            S_.dma("sp", v[half * 64:(half + 1) * 64, self.V_AQ:self.V_AQ + 1], col(I["a_q_norm"][l]), writes=[b])
            S_.dma("sp", v[half * 64:(half + 1) * 64, self.V_AK:self.V_AK + 1], col(I["a_k_norm"][l]), writes=[b])
        S_.dma("sp", v[:, self.V_CQ:self.V_CQ + 1], col(I["c_q_norm"][l][0:128]), writes=[b])
        S_.dma("sp", v[0:64, self.V_CQ + 1:self.V_CQ + 2], col(I["c_q_norm"][l][128:192]), writes=[b])
        S_.dma("sp", v[:, self.V_CKV:self.V_CKV + 1], col(I["c_kv_norm"][l]), writes=[b])
        for nm, c0, n in (("d_conv_b", self.V_DCB, 2), ("d_ln_g", self.V_DLG, 2), ("d_ln_b", self.V_DLB, 2),
                          ("ln_mix_g", self.V_LMG, 8), ("ln_mix_b", self.V_LMB, 8), ("ln_ffn_g", self.V_LFG, 8),
                          ("ln_ffn_b", self.V_LFB, 8)):
            for c in range(n):
                S_.dma("sp", v[:, c0 + c:c0 + c + 1], col(I[nm][l][c * 128:(c + 1) * 128]), writes=[b])

    def branch_A(self, l):
        I, S_ = self.I, self.S
        base = self.PH
        QA = [self.at(f"QA{h}", [128, S], BF16, base + h * 4096) for h in range(4)]
        KA = self.at("KA", [128, S], BF16, base + 16384)
        VA = self.at("VA", [128, NT, 2, 65], BF16, base + 20480)
        bQ, bK, bV = [self.nb(f"QA{h}") for h in range(4)], self.nb("KA"), self.nb("VA")
        o = base + 20480 + 4160
        for h in range(4):
            S_.op("dve", lambda e, h=h: e.memset(QA[h][:], 0.0), writes=[bQ[h]])
        rope = self.ring_at("ropeA", [128, 2, 512], F32, o, 2, 4096); o += 8192
        PT = self.ring_at("PTa", [128, 512], BF16, o, 3, 1024); o += 3072
        ytm = self.ring_at("ytmA", [128, 256], F32, o, 2, 1024); o += 2048
        assert o <= self.SCR_END
        S_.op("dve", lambda e: e.memset(VA[:, :, :, 64:65], 1.0), writes=[bV])
        gq = self.vec[:, self.V_AQ:self.V_AQ + 1]
        gk = self.vec[:, self.V_AK:self.V_AK + 1]
        for g in range(NG):
            w, wb = self.wload_segs(I["w_in"][l], KC, [(0, 64), (128, 64), (64, 64), (192, 64), (256, 256)])
            rt, brt = rope.get()
            S_.dma("sp", rt[:], I["ropeA"][:, :, g * 512:(g + 1) * 512], writes=[brt])
            for c in range(3):
                if c < 2:
                    pt, bp = self.proj(w, wb, slice(c * 128, (c + 1) * 128), g)
                    gcol = gq
                    dsts = [(QA[c][0:64, g * 512:(g + 1) * 512], bQ[c], slice(0, 64)),
                            (QA[c + 2][64:128, g * 512:(g + 1) * 512], bQ[c + 2], slice(64, 128))]
                else:
                    pt, bp = self.proj(w, wb, slice(256, 384), g)
                    gcol = gk
                    dsts = [(KA[:, g * 512:(g + 1) * 512], bK, slice(0, 128))]
                sq, bsq = self.tmp()
                self.act(sq[:], bsq, pt[:], [bp], AF.Square)
                qf, bqf = self.tmp()
                self.cp(qf[:], bqf, pt[:], [bp], eng="act")
                pss, bpss = self.psum()
                self.mm(pss[:], bpss, self.bd, sq[:], [self.bcst, bsq])
                r, br = self.rstd_from(pss, bpss, 64.0, RMS_EPS)
                qn, bqn = self.tmp()
                self.stt(qn[:], bqn, qf[:], gcol, r[:], [bqf, self.bvec, br], ALU.mult, ALU.mult)
                self.rope(qn, bqn, 128, rt[:, 0, :], rt[:, 1, :], brt, dsts)
            for t4 in range(4):
                tt = g * 4 + t4
                pv, bpv = self.psum()
                for kc in range(KC):
                    self.mm(pv[:, 0:128], bpv, self.XH[:, kc, tt * 128:(tt + 1) * 128], w[:, kc, 384:512], [wb, self.bX[g]],
                            start=(kc == 0), stop=(kc == KC - 1))
                self.cp(VA[:, tt, :, 0:64], bV, pv[:, 0:128].rearrange("p (a d) -> p a d", a=2), [bpv])
        self.dump("qa0", QA[0][:], bQ[0], [128, S], BF16)
        self.dump("ka", KA[:], bK, [128, S], BF16)
        if self.upto.endswith("A1"):
            return

        def qk_ops(h, tq, kt):
            return [(KA[:, kt * 128:(kt + 1) * 128], QA[h][:, tq * 128:(tq + 1) * 128])], [bK, bQ[h]]

        def v_ap(h, kt):
            return VA[:, kt, h // 2, :]
        self.attention(4, qk_ops, v_ap, bV, 0.125, 0, PT, ytm)
        self.dump("yTa", self.yT[0][:], self.byT[0], [128, 2, S], BF16)

    def branch_C(self, l):
        I, S_ = self.I, self.S
        o = self.PH
        QC = [self.at(f"QC{h}", [128, S], BF16, o + h * 4096) for h in range(4)]; o += 16384
        KC_ = [self.at(f"KC{h}", [128, S], BF16, o + h * 4096) for h in range(4)]; o += 16384
        VC = self.at("VC", [128, NT, 4, 65], BF16, o); o += 8320
        rope = self.ring_at("ropeC", [32, 2, 512], F32, o, 1, 4096); o += 4096
        PT = self.ring_at("PTc", [128, 512], BF16, o, 3, 1024); o += 3072
        ytm = self.ring_at("ytmC", [128, 256], F32, o, 2, 1024); o += 2048
        cqn = self.at("cqn", [128, 2, 512], BF16, o); o += 2048
        ckvn = self.at("ckvn", [128, 512], BF16, o); o += 1024
        wuq = self.at("wuq", [128, 2, 384], BF16, o); o += 1536
        wukv = self.at("wukv", [128, 1, 512], BF16, o); o += 1024
        assert o <= self.SCR_END, o
        bQ = [self.nb(f"QC{h}") for h in range(4)]
        bK = [self.nb(f"KC{h}") for h in range(4)]
        bV, bcqn, bckvn, bwuq, bwukv = self.nb("VC"), self.nb("cqn"), self.nb("ckvn"), self.nb("wuq"), self.nb("wukv")
        S_.op("dve", lambda e: e.memset(VC[:, :, :, 64:65], 1.0), writes=[bV])
        wpre = self.wload(I["w_in"][l][:, 1280:1632], KC, 352)
        raw, braw = self.wring.get()
        ruq = raw[:, 0:768].rearrange("p (k c) -> p k c", k=2)
        rukv = raw[:, 768:1280]
        S_.dma("pool", ruq[:, 0, :], I["c_w_uq"][l][0:128, :], writes=[braw])
        S_.dma("pool", ruq[0:64, 1, :], I["c_w_uq"][l][128:192, :], writes=[braw])
        S_.dma("pool", rukv, I["c_w_ukv"][l], writes=[braw])
        for k in range(2):
            pp = slice(0, 128) if k == 0 else slice(0, 64)
            rv = ruq[pp, k, :].rearrange("p (h c) -> p h c", h=4)
            self.cp(wuq[pp, k, 0:256].rearrange("p (h c) -> p h c", h=4), bwuq, rv[:, :, 0:64], [braw])
            self.cp(wuq[pp, k, 256:384].rearrange("p (h c) -> p h c", h=4), bwuq, rv[:, :, 64:96], [braw], eng="act")
        rkv = rukv.rearrange("p (h c) -> p h c", h=4)
        self.cp(wukv[:, 0, 0:256].rearrange("p (h c) -> p h c", h=4), bwukv, rkv[:, :, 0:64], [braw])
        self.cp(wukv[:, 0, 256:512].rearrange("p (h c) -> p h c", h=4), bwukv, rkv[:, :, 64:128], [braw], eng="act")
        gcq = self.vec[:, self.V_CQ:self.V_CQ + 2]
        gckv = self.vec[:, self.V_CKV:self.V_CKV + 1]
        for g in range(NG):
            gs = slice(g * 512, (g + 1) * 512)
            w, wb = wpre if g == 0 else self.wload(I["w_in"][l][:, 1280:1632], KC, 352)
            rt, brt = rope.get()
            S_.dma("sp", rt[:], I["ropeC"][:, :, gs], writes=[brt])
            p0, bp0 = self.proj(w, wb, slice(0, 128), g)
            p1, bp1 = self.proj(w, wb, slice(128, 192), g, m=64)
            sq0, bs0 = self.tmp()
            sq1, bs1 = self.tmp()
            self.act(sq0[:], bs0, p0[:], [bp0], AF.Square)
            self.act(sq1[0:64, :], bs1, p1[0:64, :], [bp1], AF.Square)
            pss, bpss = self.psum()
            self.mm(pss[:], bpss, self.ones, sq0[:], [self.bcst, bs0], start=True, stop=False)
            self.mm(pss[:], bpss, self.ones[0:64, :], sq1[0:64, :], [self.bcst, bs1], start=False, stop=True)
            r, br = self.rstd_from(pss, bpss, 192.0, RMS_EPS)
            self.stt(cqn[:, 0, :], bcqn, p0[:], gcq[:, 0:1], r[:], [bp0, self.bvec, br], ALU.mult, ALU.mult)
            self.stt(cqn[0:64, 1, :], bcqn, p1[0:64, :], gcq[0:64, 1:2], r[0:64, :], [bp1, self.bvec, br], ALU.mult, ALU.mult)
            p2, bp2 = self.proj(w, wb, slice(192, 320), g)
            sq2, bs2 = self.tmp()
            self.act(sq2[:], bs2, p2[:], [bp2], AF.Square)
            pss2, bpss2 = self.psum()
            self.mm(pss2[:], bpss2, self.ones, sq2[:], [self.bcst, bs2])
            r2, br2 = self.rstd_from(pss2, bpss2, 128.0, RMS_EPS)
            self.stt(ckvn[:], bckvn, p2[:], gckv, r2[:], [bp2, self.bvec, br2], ALU.mult, ALU.mult)
            p3, bp3 = self.proj(w, wb, slice(320, 352), g, m=32)
            kf, bkf = self.tmp()
            self.cp(kf[0:32, :], bkf, p3[0:32, :], [bp3])
            kr, bkr = self.tmp()
            self.rope(kf, bkf, 32, rt[:, 0, :], rt[:, 1, :], brt, kr[0:32, :], bkr)
            for h in range(4):
                self.cp(KC_[h][64:96, gs], bK[h], kr[0:32, :], [bkr], eng=("act" if h % 2 else "dve"))
            for c in range(2):
                pq, bpq = self.psum()
                self.mm(pq[:], bpq, wuq[:, 0, c * 128:(c + 1) * 128], cqn[:, 0, :], [bwuq, bcqn], start=True, stop=False)
                self.mm(pq[:], bpq, wuq[0:64, 1, c * 128:(c + 1) * 128], cqn[0:64, 1, :], [bwuq, bcqn], start=False, stop=True)
                self.cp(QC[2 * c][0:64, gs], bQ[2 * c], pq[0:64, :], [bpq], eng="act")
                self.cp(QC[2 * c + 1][0:64, gs], bQ[2 * c + 1], pq[64:128, :], [bpq])
            for h in range(4):
                pq, bpq = self.psum()
                self.mm(pq[0:32, :], bpq, wuq[:, 0, 256 + h * 32:256 + (h + 1) * 32], cqn[:, 0, :], [bwuq, bcqn], start=True, stop=False)
                self.mm(pq[0:32, :], bpq, wuq[0:64, 1, 256 + h * 32:256 + (h + 1) * 32], cqn[0:64, 1, :], [bwuq, bcqn], start=False, stop=True)
                qf, bqf = self.tmp()
                self.cp(qf[0:32, :], bqf, pq[0:32, :], [bpq])
                self.rope(qf, bqf, 32, rt[:, 0, :], rt[:, 1, :], brt, QC[h][64:96, gs], bQ[h])
            for c in range(2):
                pk, bpk = self.psum()
                self.mm(pk[:], bpk, wukv[:, 0, c * 128:(c + 1) * 128], ckvn[:], [bwukv, bckvn])
                self.cp(KC_[2 * c][0:64, gs], bK[2 * c], pk[0:64, :], [bpk], eng="act")
                self.cp(KC_[2 * c + 1][0:64, gs], bK[2 * c + 1], pk[64:128, :], [bpk])
            for t4 in range(4):
                tt = g * 4 + t4
                pv, bpv = self.psum()
                self.mm(pv[:, 0:256], bpv, ckvn[:, t4 * 128:(t4 + 1) * 128], wukv[:, 0, 256:512], [bckvn, bwukv])
                self.cp(VC[:, tt, :, 0:64], bV, pv[:, 0:256].rearrange("p (a d) -> p a d", a=4), [bpv])
        self.dump("qc0", QC[0][:], bQ[0], [128, S], BF16)
        self.dump("kc1", KC_[1][:], bK[1], [128, S], BF16)
        if self.upto.endswith("C1"):
            return

        def qk_ops(h, tq, kt):
            return ([(KC_[h][0:96, kt * 128:(kt + 1) * 128], QC[h][0:96, tq * 128:(tq + 1) * 128])], [bK[h], bQ[h]])

        def v_ap(h, kt):
            return VC[:, kt, h, :]
        self.attention(4, qk_ops, v_ap, bV, 96.0 ** -0.5, 2, PT, ytm)
        self.dump("yTc", self.yT[2][:], self.byT[2], [128, 2, S], BF16)

    def branch_B(self, l):
        I, S_ = self.I, self.S
        o = self.PH
        QB = [self.at(f"QB{h}", [128, S], BF16, o + h * 4096) for h in range(4)]; o += 16384
        KB = [self.at(f"KB{c}", [128, S], BF16, o + c * 4096) for c in range(2)]; o += 8192
        VB = self.at("VB", [128, NT, 4, 65], BF16, o); o += 8320
        VBs = self.at("VBs", [128, NT - 1, 4, 65], BF16, o); o += 7808
        tbl = self.at("tblB", [128, 4, 14, 64], BF16, o); o += 7168
        PT = self.ring_at("PTb", [128, 4, 4, 64], BF16, o, 3, 2048); o += 6144
        ytm = self.ring_at("ytmB", [128, 256], F32, o, 2, 1024); o += 2048
        assert o <= self.SCR_END, o
        bQ, bK = [self.nb(f"QB{h}") for h in range(4)], [self.nb("KB0"), self.nb("KB1")]
        for h in range(4):
            S_.op("dve", lambda e, h=h: e.memset(QB[h][:], 0.0), writes=[bQ[h]])
        bV, bVs, btbl = self.nb("VB"), self.nb("VBs"), self.nb("tblB")
        S_.op("dve", lambda e: e.memset(VB[:, :, :, 64:65], 1.0), writes=[bV])
        S_.op("dve", lambda e: e.memset(VBs[:, :, :, 64:65], 1.0), writes=[bVs])
        for h in range(4):
            for p7 in range(2):
                tf, btf = self.tmp()
                tfv = tf[:, 0:448].rearrange("p (a b) -> p a b", a=7)
                for pj in range(7):
                    pi = p7 * 7 + pj
                    S_.dma("sp", tfv[:, pj, :], I["rpbT"][l, h, pi * 64:pi * 64 + 128, :], writes=[btf])
                for pj in range(7):
                    pi = p7 * 7 + pj
                    self.tt(tbl[:, h, pi, :], btbl, tfv[:, pj, :], self.maskB, [btf, self.bcst], ALU.add)
        for g in range(NG):
            gs = slice(g * 512, (g + 1) * 512)
            w, wb = self.wload(I["w_in"][l][:, 512:1024], KC, 512)
            wv, wvb = self.wload(I["w_in"][l][:, 1024:1280], KC, 256)
            for c in range(2):
                pq, bpq = self.proj(w, wb, slice(c * 128, (c + 1) * 128), g)
                for h2 in range(2):
                    self.cp(QB[2 * c + h2][h2 * 64:(h2 + 1) * 64, gs], bQ[2 * c + h2], pq[h2 * 64:(h2 + 1) * 64, :], [bpq],
                            eng="act", scale=0.125)
                pk, bpk = self.proj(w, wb, slice(256 + c * 128, 256 + (c + 1) * 128), g)
                self.cp(KB[c][:, gs], bK[c], pk[:], [bpk])
            for t4 in range(4):
                tt = g * 4 + t4
                for sh in range(2):
                    if sh == 1 and tt == NT - 1:
                        continue
                    t0 = tt * 128 + sh * 64
                    rb = [wvb, self.bX[g]] + ([self.bX[g + 1]] if (sh == 1 and t4 == 3) else [])
                    pv, bpv = self.psum()
                    for kc in range(KC):
                        self.mm(pv[:, 0:256], bpv, self.XH[:, kc, t0:t0 + 128], wv[:, kc, :], rb,
                                start=(kc == 0), stop=(kc == KC - 1))
                    dstV, bdst = (VB, bV) if sh == 0 else (VBs, bVs)
                    self.cp(dstV[:, tt, :, 0:64], bdst, pv[:, 0:256].rearrange("p (a d) -> p a d", a=4), [bpv],
                            eng=("act" if sh else "dve"))
        rows = list(range(2 * NT))

        def rs_of(r):
            return min(max(r - 4, 0), 24)

        def emit_qk(r):
            rs = rs_of(r)
            k0 = rs * 64
            out = []
            for hp in range(2):
                ps_, bps = self.psum()
                for h2 in range(2):
                    h = hp * 2 + h2
                    for j in range(4):
                        reg = ps_[:, h2 * 256 + j * 64:h2 * 256 + (j + 1) * 64]
                        self.mm(reg, bps, KB[hp][:, k0 + 128 * j:k0 + 128 * (j + 1)], QB[h][:, r * 64:(r + 1) * 64],
                                [bK[hp], bQ[h]])
                out.append((ps_, bps))
            return out

        cur = {}

        def emit_rest(r, banks):
            tq, rr = r // 2, r % 2
            rs = rs_of(r)
            if rr == 0:
                cur["ytm"], cur["bytm"] = ytm.get()
                cur["po"], cur["bpo"] = self.aring.get()
            po, bpo = cur["po"], cur["bpo"]
            pt_, bpt = PT.get()
            for hp in range(2):
                ps_, bps = banks[hp]
                sc, bsc = self.tmp()
                pi0 = rs - r + 7
                for h2 in range(2):
                    h = hp * 2 + h2
                    self.tt(sc[:, h2 * 256:(h2 + 1) * 256].rearrange("p (a b) -> p a b", a=4),
                            bsc, ps_[:, h2 * 256:(h2 + 1) * 256].rearrange("p (a b) -> p a b", a=4),
                            tbl[:, h, pi0:pi0 + 7:2, :], [bps, btbl], ALU.add)
                self.act(pt_[:, hp * 2:hp * 2 + 2, :, :], bpt, sc[:].rearrange("p (a b c) -> p a b c", a=2, b=4), [bsc], AF.Exp)
            for h in range(4):
                for j in range(4):
                    if rs % 2 == 0:
                        vt, bvt = VB[:, rs // 2 + j, h, :], bV
                    else:
                        vt, bvt = VBs[:, (rs - 1) // 2 + j, h, :], bVs
                    self.mm(po[rr * 64:(rr + 1) * 64, h * 65:(h + 1) * 65], bpo, pt_[:, h, j, :], vt, [bpt, bvt],
                            start=(j == 0), stop=(j == 3))
            if rr == 1:
                self.norm_out(po, bpo, 4, cur["ytm"], cur["bytm"], 1, tq)

        nxt = emit_qk(rows[0])
        for i, r in enumerate(rows):
            this = nxt
            nxt = emit_qk(rows[i + 1]) if i + 1 < len(rows) else None
            emit_rest(r, this)
        self.dump("yTb", self.yT[1][:], self.byT[1], [128, 2, S], BF16)

    def branch_D(self, l):
        I, S_ = self.I, self.S
        o = self.PH
        uT = self.at("uT", [128, 2, S + 30], BF16, o); o += 2 * (S + 30) * 2 + 8
        Dg = self.at("Dg", [128, 31, 128], BF16, o); o += 31 * 128 * 2
        vT = self.at("vT", [128, 2, S], F32, o); o += 2 * S * 4
        cw = self.at("cw", [128, 256], F32, o); o += 1024
        wc = self.at("wc", [128, 2, 32], F32, o); o += 256
        assert o <= self.SCR_END, o
        buT, bDg, bvT, bcw, bwc = self.nb("uT"), self.nb("Dg"), self.nb("vT"), self.nb("cw"), self.nb("wc")
        S_.op("dve", lambda e: e.memset(uT[:, :, 0:15], 0.0), writes=[buT])
        S_.op("dve", lambda e: e.memset(uT[:, :, S + 15:S + 30], 0.0), writes=[buT])
        S_.dma("sp", cw[0:31, :], I["d_conv_w"][l], writes=[bcw])
        pt, bp = self.psum()
        for ct in range(2):
            self.tr(pt[:, ct * 32:ct * 32 + 31], bp, cw[0:31, ct * 128:(ct + 1) * 128], self.ident[0:31, 0:31], [bcw, self.bcst])
        self.cp(wc[:, :, 0:31], bwc, pt[:, 0:64].rearrange("p (a b) -> p a b", a=2)[:, :, 0:31], [bp])
        for g in range(NG):
            w, wb = self.wload(I["w_in"][l][:, 1632:2144], KC, 512)
            for ct in range(2):
                pa, bpa = self.proj(w, wb, slice(ct * 128, (ct + 1) * 128), g)
                pb, bpb = self.proj(w, wb, slice(256 + ct * 128, 256 + (ct + 1) * 128), g)
                sg, bsg = self.tmp()
                self.act(sg[:], bsg, pb[:], [bpb], AF.Sigmoid)
                self.tt(uT[:, ct, 15 + g * 512:15 + (g + 1) * 512], buT, pa[:], sg[:], [bpa, bsg], ALU.mult)
        for ct in range(2):
            for j in range(31):
                self.ts(Dg[:, j, :], bDg, self.idb[:], wc[:, ct, j:j + 1], None, [self.bcst, bwc], ALU.mult)
            for g in range(NG):
                pc, bpc = self.psum()
                for j in range(31):
                    self.mm(pc[:], bpc, Dg[:, j, :], uT[:, ct, g * 512 + j:g * 512 + j + 512], [bDg, buT],
                            start=(j == 0), stop=(j == 30))
                self.act(vT[:, ct, g * 512:(g + 1) * 512], bvT, pc[:], [bpc, self.bvec], AF.Identity,
                         bias=self.vec[:, self.V_DCB + ct:self.V_DCB + ct + 1])
        for g in range(NG):
            gs = slice(g * 512, (g + 1) * 512)
            s1, bs1 = self.psum()
            s2, bs2 = self.psum()
            for ct in range(2):
                self.mm(s1[:], bs1, self.ones, vT[:, ct, gs], [self.bcst, bvT], start=(ct == 0), stop=(ct == 1))
            for ct in range(2):
                sq, bsq = self.tmp()
                self.act(sq[:], bsq, vT[:, ct, gs], [bvT], AF.Square)
                self.mm(s2[:], bs2, self.ones, sq[:], [self.bcst, bsq], start=(ct == 0), stop=(ct == 1))
            mean, bmean = self.lnm, self.blnm
            self.act(mean[:], bmean, s1[:], [bs1], AF.Copy, scale=1.0 / 256)
            var, bvar = self.lnv, self.blnv
            self.act(var[:], bvar, s1[:], [bs1], AF.Square, scale=1.0 / 256)
            self.stt(var[:], bvar, s2[:], 1.0 / 256, var[:], [bs2, bvar], ALU.mult, ALU.subtract)
            self.act(var[:], bvar, var[:], [bvar], AF.Ln, bias=LN_EPS)
            self.act(var[:], bvar, var[:], [bvar], AF.Exp, scale=-0.5)
            self.stt(mean[:], bmean, mean[:], -1.0, var[:], [bmean, bvar], ALU.mult, ALU.mult)
            for ct in range(2):
                u, bu = self.tmp()
                self.tt(u[:], bu, vT[:, ct, gs], var[:], [bvT, bvar], ALU.mult)
                self.tt(u[:], bu, u[:], mean[:], [bu, bmean], ALU.add)
                yT3 = self.yT[3]
                self.S.op("act", lambda e, u=u, ct=ct, gs=gs, yT3=yT3: e.activation(
                    out=yT3[:, ct, gs], in_=u[:], func=AF.Silu,
                    scale=self.vec[:, self.V_DLG + ct:self.V_DLG + ct + 1],
                    bias=self.vec[:, self.V_DLB + ct:self.V_DLB + ct + 1]), reads=[bu, self.bvec], writes=[self.byT[3]])
        self.dump("yTd", self.yT[3][:], self.byT[3], [128, 2, S], BF16)

    def merge(self, l):
        I, S_ = self.I, self.S
        o = self.PH
        merged = self.at("merged", [128, KC, 1024], BF16, o); o += 16384
        macc = self.at("macc", [128, 8, 512], F32, o); o += 16384
        tT = self.at("tT", [128, KC, 512], F32, o); o += 16384
        assert o <= self.SCR_END, o
        bmer, bmacc, btT = self.nb("merged"), [self.nb(f"macc{i}") for i in range(8)], self.nb("tT")
        for th in range(2):
            for ocq in range(2):
                for n in range(4):
                    c0 = 2144 + n * 1024 + ocq * 512
                    wg, bwg = self.wload(I["w_in"][l][:, c0:c0 + 512], KC, 512)
                    wbr, bwbr = self.wload(I["w_branch"][l, n][:, ocq * 512:(ocq + 1) * 512], 2, 512)
                    for oci in range(4):
                        for gi in range(2):
                            g = 2 * th + gi
                            idx = oci * 2 + gi
                            oc = ocq * 4 + oci
                            pg, bpg = self.proj(wg, bwg, slice(oci * 128, (oci + 1) * 128), g)
                            pb, bpb = self.psum()
                            for k2 in range(2):
                                self.mm(pb[:], bpb, wbr[:, k2, oci * 128:(oci + 1) * 128], self.yT[n][:, k2, g * 512:(g + 1) * 512],
                                        [bwbr, self.byT[n]], start=(k2 == 0), stop=(k2 == 1))
                            sg, bsg = self.tmp()
                            self.act(sg[:], bsg, pg[:], [bpg], AF.Sigmoid)
                            if n == 0:
                                self.tt(macc[:, idx, :], bmacc[idx], pb[:], sg[:], [bpb, bsg], ALU.mult)
                            else:
                                self.tt(sg[:], bsg, pb[:], sg[:], [bpb, bsg], ALU.mult)
                                if n < 3:
                                    self.tt(macc[:, idx, :], bmacc[idx], macc[:, idx, :], sg[:], [bmacc[idx], bsg], ALU.add)
                                else:
                                    self.tt(merged[:, oc, gi * 512:(gi + 1) * 512], bmer, macc[:, idx, :], sg[:], [bmacc[idx], bsg],
                                            ALU.add)
            if th == 0:
                self.dump("merged0", merged[:], bmer, [128, KC, 1024], BF16)
            for gi in range(2):
                g = 2 * th + gi
                gs = slice(g * 512, (g + 1) * 512)
                for half in range(2):
                    wo, bwo = self.wload(I["w_out"][l][:, half * 512:(half + 1) * 512], KC, 512)
                    for oi in range(4):
                        oc2 = half * 4 + oi
                        po, bpo = self.psum()
                        for fc in range(KC):
                            self.mm(po[:], bpo, wo[:, fc, oi * 128:(oi + 1) * 128], merged[:, fc, gi * 512:(gi + 1) * 512], [bwo, bmer],
                                    start=(fc == 0), stop=(fc == KC - 1))
                        self.stt(tT[:, oc2, :], btT, self.XH[:, oc2, gs], ALPHA, po[:], [self.bX[g], bpo], ALU.mult, ALU.add)
                        self.stt(tT[:, oc2, :], btT, self.XL[:, oc2, gs], ALPHA, tT[:, oc2, :], [self.bX[g], btT], ALU.mult, ALU.add)
                if g == 0:
                    self.dump("tT0", tT[:], btT, [128, KC, 512])
                self.ln_fm(tT, btT, self.V_LMG, self.V_LMB, g)
        self.dump(f"x1h{l}", self.XH[:], self.bX[3], [128, KC, S], BF16)
        self.dump(f"x1l{l}", self.XL[:], self.bX[3], [128, KC, S], BF16)

    def ffn(self, l):
        I, S_ = self.I, self.S
        moe = (l % 2 == 1)
        final = (l == DEPTH - 1)
        o = self.SCR
        acc = self.at("acc", [128, KC, 1024], F32, o); o += 32768
        hring = self.ring_at("hT", [128, 4, 1024], BF16, o, 2, 8192); o += 16384
        gBr = self.ring_at("gB", [128, 1024], F32, o, 2, 4096); o += 8192
        self.oring = self.ring_at("ostg", [128, 4, 128], F32, o, 2, 2048); o += 4096
        gtm = self.at("gtm", [128, NT, 8], F32, o); o += 512
        rt32 = self.at("rt32", [128, KC, 8], F32, o); o += 256
        rth = self.at("rth", [128, KC, 8], BF16, o); o += 128
        rtl = self.at("rtl", [128, KC, 8], BF16, o); o += 128
        lg = self.at("lg", [128, 16], F32, o); o += 64
        nextra = (self.SCR_END - o) // 8192
        extra = [(self.at("wsx", [128, 4096], BF16, o + i * 8192), self.nb(f"wsx{i}")) for i in range(nextra)]
        self.wring = MK.Ring(self.wring4.items + extra)
        bacc, bgtm, brt, blg = self.nb("acc"), self.nb("gtm"), self.nb("rt"), self.nb("lg")
        if moe:
            e0 = l // 2
            S_.dma("sp", rt32[:], I["moe_router"][e0].rearrange("(k p) e -> p k e", p=128), writes=[brt])
            self.cp(rth[:], brt, rt32[:], [brt])
            self.tt(rtl[:], brt, rt32[:], rth[:], [brt], ALU.subtract)
            for tt in range(NT):
                g = tt // 4
                ts_ = slice(tt * 128, (tt + 1) * 128)
                pl, bpl = self.psum()
                n = 0
                for kc in range(KC):
                    for (xa, ra) in ((self.XH, rth), (self.XH, rtl), (self.XL, rth)):
                        self.mm(pl[:, 0:8], bpl, xa[:, kc, ts_], ra[:, kc, :], [self.bX[g], brt], start=(n == 0), stop=(n == 3 * KC - 1))
                        n += 1
                self.cp(lg[:, 0:8], blg, pl[:, 0:8], [bpl])
                if tt == 0:
                    self.dump("lg0", lg[:, 0:8], blg, [128, 8])
                S_.op("dve", lambda e: e.max(out=lg[:, 8:16], in_=lg[:, 0:8]), reads=[blg], writes=[blg])
                sm = self.sm
                self.tt(sm[:, 32:33], self.bsm, lg[:, 9:10], lg[:, 8:9], [blg], ALU.subtract)
                self.act(sm[:, 33:34], self.bsm, sm[:, 32:33], [self.bsm], AF.Exp)
                self.ts(sm[:, 34:35], self.bsm, sm[:, 33:34], 1.0, None, [self.bsm], ALU.add)
                S_.op("dve", lambda e: e.reciprocal(out=sm[:, 35:36], in_=sm[:, 34:35]), reads=[self.bsm], writes=[self.bsm])
                self.tt(sm[:, 36:37], self.bsm, sm[:, 33:34], sm[:, 35:36], [self.bsm], ALU.mult)
                self.ts(gtm[:, tt, :], bgtm, lg[:, 0:8], lg[:, 8:9], sm[:, 35:36], [blg, self.bsm], ALU.is_equal, ALU.mult)
                self.ts(sm[:, 40:48], self.bsm, lg[:, 0:8], lg[:, 9:10], sm[:, 36:37], [blg, self.bsm], ALU.is_equal, ALU.mult)
                self.tt(gtm[:, tt, :], bgtm, gtm[:, tt, :], sm[:, 40:48], [bgtm, self.bsm], ALU.add)
            self.dump("gtm", gtm[:], bgtm, [128, NT, 8])
            w1a, w3a, w2a = I["moe_w1"][e0], I["moe_w3"][e0], I["moe_w2"][e0]
            nff, experts = D_FFE // 128, list(range(NE))
        else:
            e0 = l // 2
            w1a, w3a, w2a = I["ffn_w1"], I["ffn_w3"], I["ffn_w2"]
            nff, experts = D_FF // 128, [e0]
        blocks = [(f0, min(4, nff - f0)) for f0 in range(0, nff, 4)]
        for th in range(2):
            work = [(e, f0, nf) for e in experts for (f0, nf) in blocks]
            gBs = {}

            def h_phase(item):
                e, f0, nf = item
                if moe and e not in gBs:
                    gB, bgB = gBr.get()
                    for t8 in range(8):
                        tt = th * 8 + t8
                        if t8 % 4 == 0:
                            pgb, bpgb = self.psum()
                        rep, brep = self.tmp()
                        self.ts(rep[:, 0:128], brep, self.ones, gtm[:, tt, e:e + 1], None, [self.bcst, bgtm], ALU.mult)
                        self.mm(pgb[:, (t8 % 4) * 128:(t8 % 4 + 1) * 128], bpgb, rep[:, 0:128], self.ident, [brep, self.bcst])
                        if t8 % 4 == 3:
                            self.cp(gB[:, (t8 // 4) * 512:(t8 // 4 + 1) * 512], bgB, pgb[:], [bpgb], eng="act")
                    gBs[e] = (gB, bgB)
                w1s, bw1 = self.wload(w1a[e][:, f0 * 128:(f0 + nf) * 128], KC, nf * 128)
                w3s, bw3 = self.wload(w3a[e][:, f0 * 128:(f0 + nf) * 128], KC, nf * 128)
                w2s, bw2 = self.wload(w2a[e][f0 * 128:(f0 + nf) * 128, :], nf, 1024)
                hT, bh = hring.get()
                for f in range(nf):
                    for gi in range(2):
                        g = 2 * th + gi
                        p1, bp1 = self.proj(w1s, bw1, slice(f * 128, (f + 1) * 128), g)
                        p3, bp3 = self.proj(w3s, bw3, slice(f * 128, (f + 1) * 128), g)
                        s_, bs_ = self.tmp()
                        self.act(s_[:], bs_, p1[:], [bp1], AF.Silu)
                        if moe:
                            gB, bgB = gBs[e]
                            self.tt(s_[:], bs_, s_[:], gB[:, gi * 512:(gi + 1) * 512], [bs_, bgB], ALU.mult)
                        self.tt(hT[:, f, gi * 512:(gi + 1) * 512], bh, p3[:], s_[:], [bp3, bs_], ALU.mult)
                return (nf, w2s, bw2, hT, bh)

            def w2_phase(st, first):
                nf, w2s, bw2, hT, bh = st
                for oc in range(KC):
                    for gi in range(2):
                        g = 2 * th + gi
                        gs = slice(g * 512, (g + 1) * 512)
                        po, bpo = self.psum()
                        for f in range(nf):
                            self.mm(po[:], bpo, w2s[:, f, oc * 128:(oc + 1) * 128], hT[:, f, gi * 512:(gi + 1) * 512], [bw2, bh],
                                    start=(f == 0), stop=(f == nf - 1))
                        a_ = acc[:, oc, gi * 512:(gi + 1) * 512]
                        if first:
                            self.stt(a_, bacc, self.XH[:, oc, gs], ALPHA, po[:], [self.bX[g], bpo], ALU.mult, ALU.add)
                            self.stt(a_, bacc, self.XL[:, oc, gs], ALPHA, a_, [self.bX[g], bacc], ALU.mult, ALU.add)
                        else:
                            self.tt(a_, bacc, po[:], a_, [bpo, bacc], ALU.add)

            prev = h_phase(work[0])
            for i in range(len(work)):
                nxt = h_phase(work[i + 1]) if i + 1 < len(work) else None
                w2_phase(prev, first=(i == 0))
                prev = nxt
            for gi in range(2):
                g = 2 * th + gi
                self.ln_fm(acc[:, :, gi * 512:(gi + 1) * 512], bacc, self.V_LFG, self.V_LFB, g, final=final)
        if not final:
            self.dump(f"x2h{l}", self.XH[:], self.bX[3], [128, KC, S], BF16)
        self.phase_bufs.extend([b for (_, b) in self.wring4.items])
        self.wring = self.wring4


def host_inputs(inputs, b):
    m = {}
    m["x"] = np.ascontiguousarray(inputs["x"][b])
    for k in ("ln_in_g", "ln_in_b", "w_in", "a_q_norm", "a_k_norm", "c_q_norm", "c_kv_norm", "c_w_uq", "c_w_ukv",
              "d_conv_b", "d_ln_g", "d_ln_b", "w_branch", "w_out", "ln_mix_g", "ln_mix_b", "ffn_w1", "ffn_w3",
              "ffn_w2", "moe_router", "moe_w1", "moe_w3", "moe_w2", "ln_ffn_g", "ln_ffn_b"):
        m[k] = np.asarray(inputs[k], dtype=np.float32)
    m["d_conv_w"] = np.asarray(inputs["d_conv_w"], dtype=np.float32).reshape(DEPTH, 31, 256)
    cc = np.arange(GRID_W)
    dc = np.clip(cc[:, None] - cc[None, :] + 15, 0, 30)
    rpb = np.asarray(inputs["b_rpb"], dtype=np.float32)
    m["rpbT"] = np.ascontiguousarray(rpb[:, :, :, dc].reshape(DEPTH, 4, 15 * 64, 64))
    return m


_CONSTS = None


def consts():
    global _CONSTS
    if _CONSTS is None:
        _CONSTS = {"cst": make_consts(), "ropeA": make_rope(64, 2), "ropeC": make_rope(32, 1)}
    return _CONSTS


def kernel(**inputs):
    mk = MK()
    maps = []
    shared = None
    for b in range(8):
        m = host_inputs(inputs, b)
        m.update(consts())
        maps.append(m)
    res = run_bass_kernel_spmd(mk.nc, maps, core_ids=list(range(8)))
    out = np.stack([np.asarray(r["out"]) for r in res.results], axis=0)
    return out.astype(np.float32)
```

```python
import numpy as np
import concourse.bass as bass
import concourse.mybir as mybir
from concourse.bass_utils import run_bass_kernel_spmd

F32 = mybir.dt.float32
BF16 = mybir.dt.bfloat16
AF = mybir.ActivationFunctionType
ALU = mybir.AluOpType

D = 1024
S = 2048
NT = 16
NG = 4
KC = 8
DEPTH = 2
GRID_W = 64
D_IN = 6240
D_FF = 2816
NE = 8
D_FFE = 3584
ALPHA = (2 * DEPTH) ** 0.25
RMS_EPS = 1e-6
LN_EPS = 1e-5
NEG = -30000.0

ENGS = ("pe", "act", "dve", "pool", "sp")


class Buf:
    __slots__ = ("name", "writer", "readers", "dsem", "dcount", "excl")

    def __init__(self, name, excl=False):
        self.excl = excl
        self.name = name
        self.writer = None
        self.readers = {}
        self.dsem = None
        self.dcount = 0


class Ins:
    __slots__ = ("eng", "fn", "deps", "signal", "sem", "val", "is_dma")

    def __init__(self, eng, fn):
        self.eng = eng
        self.fn = fn
        self.deps = []
        self.signal = False
        self.sem = None
        self.val = None
        self.is_dma = False


class Sched:
    def __init__(self, nc):
        self.nc = nc
        self.q = {e: [] for e in ENGS}
        self.esem = {}
        self.shared_dsem = None

    def _add(self, eng, fn, reads, writes, is_dma=False, dma_buf=None):
        ins = Ins(eng, fn)
        ins.is_dma = is_dma
        writes = writes + [r for r in reads if r.excl and r not in writes]
        deps = []
        for r in reads:
            if r.writer is not None:
                deps.append(r.writer)
        for w in writes:
            if w.writer is not None:
                deps.append(w.writer)
            deps.extend(w.readers.values())
        seen = set()
        for d in deps:
            if d is ins or id(d) in seen:
                continue
            seen.add(id(d))
            if (not d.is_dma) and (not is_dma) and d.eng == "pe" and eng == "pe":
                continue
            ins.deps.append(d)
            d.signal = True
        if is_dma:
            b = dma_buf
            if b.dsem is None:
                b.dsem = self.nc.alloc_semaphore("d_" + b.name)
            b.dcount += 16
            ins.sem = b.dsem
            ins.val = b.dcount
            ins.signal = True
        for w in writes:
            w.writer = ins
            w.readers = {}
        for r in reads:
            if r in writes:
                continue
            r.readers[("dma", id(ins)) if is_dma else eng] = ins
        self.q[eng].append(ins)
        return ins

    def op(self, eng, fn, reads=(), writes=()):
        return self._add(eng, fn, list(reads), list(writes))

    def dma(self, eng, out, in_, reads=(), writes=(), track=None):
        reads = list(reads)
        writes = list(writes)
        b = track if track is not None else (writes[0] if writes else reads[0])

        def fn(e, out=out, in_=in_):
            return e.dma_start(out=out, in_=in_)
        return self._add(eng, fn, reads, writes, is_dma=True, dma_buf=b)

    def emit(self, final_waits=()):
        nc = self.nc
        for e in ENGS:
            self.esem[e] = nc.alloc_semaphore("e_" + e)
        for e in ENGS:
            t = 0
            for ins in self.q[e]:
                if ins.is_dma:
                    continue
                if ins.signal:
                    t += 1
                    ins.sem = self.esem[e]
                    ins.val = t
        sched = self

        def run(engname, eng):
            waited = {}
            for ins in sched.q[engname]:
                for d in ins.deps:
                    key = id(d.sem)
                    if waited.get(key, 0) >= d.val:
                        continue
                    waited[key] = d.val
                    eng.wait_ge(d.sem, d.val)
                bi = ins.fn(eng)
                if ins.is_dma:
                    bi.then_inc(ins.sem, 16)
                elif ins.signal:
                    bi.then_inc(ins.sem, 1)
            if engname == "sp":
                for b in final_waits:
                    if b.dsem is not None and b.dcount > 0:
                        eng.wait_ge(b.dsem, b.dcount)

        with nc.Block() as block:
            @block.tensor
            def _(e):
                run("pe", e)

            @block.scalar
            def _(e):
                run("act", e)

            @block.vector
            def _(e):
                run("dve", e)

            @block.gpsimd
            def _(e):
                run("pool", e)

            @block.sync
            def _(e):
                run("sp", e)


C_ID, C_ONES, C_BD, C_RM, C_MASK, C_W = 0, 128, 256, 384, 512, 576


def make_consts():
    c = np.zeros((128, C_W), np.float32)
    c[:, C_ID:C_ID + 128] = np.eye(128, dtype=np.float32)
    c[:, C_ONES:C_ONES + 128] = 1.0
    bd = np.zeros((128, 128), np.float32)
    bd[:64, :64] = 1.0
    bd[64:, 64:] = 1.0
    c[:, C_BD:C_BD + 128] = bd
    rm = np.zeros((128, 128), np.float32)
    for i in range(64):
        rm[2 * i + 1, 2 * i] = -1.0
        rm[2 * i, 2 * i + 1] = 1.0
    c[:, C_RM:C_RM + 128] = rm
    cc = np.arange(GRID_W)
    col_start = np.clip(cc - 8, 0, GRID_W - 16)
    ok = (cc[None, :] >= col_start[:, None]) & (cc[None, :] < col_start[:, None] + 16)
    m = np.where(ok.T, 0.0, NEG).astype(np.float32)
    c[:64, C_MASK:C_MASK + 64] = m
    c[64:, C_MASK:C_MASK + 64] = m
    return c


def make_rope(rot_dim, reps):
    t = np.arange(S)
    row = (t // GRID_W).astype(np.float32)
    col = (t % GRID_W).astype(np.float32)
    n_freq = rot_dim // 4
    inv = (10000.0 ** (-np.arange(n_freq, dtype=np.float32) / n_freq)).astype(np.float32)
    ang = np.concatenate([row[:, None] * inv, col[:, None] * inv], axis=-1)
    cos = np.cos(ang).astype(np.float32)
    sin = np.sin(ang).astype(np.float32)
    cosT = np.repeat(cos.T, 2, axis=0)
    sinT = np.repeat(sin.T, 2, axis=0)
    out = np.stack([np.tile(cosT, (reps, 1)), np.tile(sinT, (reps, 1))], axis=1)
    return np.ascontiguousarray(out.astype(np.float32))


class MK:
    def __init__(self, upto="all", debug=(), light=False, skip=""):
        self.light = light
        self.skip = skip
        self.upto = upto
        self.debug = set(debug)
        self.nc = bass.Bass("TRN2", target_bir_lowering=False)
        self.S = Sched(self.nc)
        self.dbg_out = {}
        self._uid = 0
        self.phase_bufs = []
        self.cur_barrier = None
        self.build()

    def uid(self, p):
        self._uid += 1
        return f"{p}{self._uid}"

    def at(self, name, shape, dtype, off):
        return self.nc.alloc_sbuf_tensor_at(self.uid(name), list(shape), dtype, offset=off)

    def din(self, name, shape, dtype=F32):
        return self.nc.dram_tensor(name, list(shape), dtype, kind="ExternalInput").ap()

    def nb(self, name):
        b = Buf(self.uid(name))
        b.writer = self.cur_barrier
        self.phase_bufs.append(b)
        return b

    def barrier(self):
        bufs = self.phase_bufs
        junk = self.junk
        ins = self.S.op("dve", lambda e: e.memset(junk[:, 0:1], 0.0), reads=[], writes=bufs + [self.bjunk])
        self.cur_barrier = ins
        self.phase_bufs = []

    class Ring:
        def __init__(self, items):
            self.items = items
            self.i = 0

        def get(self):
            it = self.items[self.i % len(self.items)]
            self.i += 1
            return it

    def ring_at(self, name, shape, dtype, off, n, nbytes, phase=True):
        return MK.Ring([(self.at(name, shape, dtype, off + i * nbytes), self.nb(f"{name}{i}") if phase else Buf(f"{name}{i}"))
                        for i in range(n)])

    def _spacer(self, is32):
        if (not is32) and getattr(self, "last_fp32", False) and self.use_spacer:
            spb, idb = self.spb, self.idb
            self.S.op("pe", lambda e: e.matmul(spb[0:32, 0:32], lhsT=idb[:, 0:32], rhs=idb[:, 0:32], start=True, stop=True),
                      reads=[self.bcst], writes=[self.bspb])
        self.last_fp32 = is32

    def mm(self, out, ob, lhsT, rhs, rb, start=True, stop=True):
        self._spacer(lhsT.dtype == F32)
        self.S.op("pe", lambda e: e.matmul(out, lhsT=lhsT, rhs=rhs, start=start, stop=stop), reads=rb, writes=[ob])

    def tr(self, out, ob, in_, ident, rb):
        self._spacer(in_.dtype == F32)
        self.S.op("pe", lambda e: e.transpose(out, in_, ident), reads=rb, writes=[ob])

    def act(self, out, ob, in_, rb, func, scale=1.0, bias=0.0):
        self.S.op("act", lambda e: e.activation(out=out, in_=in_, func=func, bias=bias, scale=scale),
                  reads=rb, writes=[ob])

    def tt(self, out, ob, in0, in1, rb, op, eng="dve"):
        self.S.op(eng, lambda e: e.tensor_tensor(out=out, in0=in0, in1=in1, op=op), reads=rb, writes=[ob])

    def ts(self, out, ob, in0, s1, s2, rb, op0, op1=None, eng="dve"):
        if op1 is None:
            self.S.op(eng, lambda e: e.tensor_scalar(out=out, in0=in0, scalar1=s1, scalar2=None, op0=op0),
                      reads=rb, writes=[ob])
        else:
            self.S.op(eng, lambda e: e.tensor_scalar(out=out, in0=in0, scalar1=s1, scalar2=s2, op0=op0, op1=op1),
                      reads=rb, writes=[ob])

    def stt(self, out, ob, in0, sc, in1, rb, op0, op1):
        self.S.op("dve", lambda e: e.scalar_tensor_tensor(out=out, in0=in0, scalar=sc, in1=in1, op0=op0, op1=op1),
                  reads=rb, writes=[ob])

    def cp(self, out, ob, in_, rb, eng="dve", scale=None):
        if eng == "act":
            sc = 1.0 if scale is None else scale
            self.S.op("act", lambda e: e.activation(out=out, in_=in_, func=AF.Copy, scale=sc), reads=rb, writes=[ob])
        else:
            self.S.op(eng, lambda e: e.tensor_copy(out=out, in_=in_), reads=rb, writes=[ob])

    def dump(self, key, ap, buf, shape, dtype=F32):
        if key not in self.debug:
            return
        d = self.nc.dram_tensor("dbg_" + key, list(shape), dtype, kind="ExternalOutput").ap()
        b = Buf("dbg_" + key)
        self.S.dma("sp", d, ap, reads=[buf], track=b)
        self.dbg_out[key] = b

    def wload(self, src_rows, nk, ncols):
        t, b = self.wring.get()
        v = t[:, 0:nk * ncols].rearrange("p (k c) -> p k c", k=nk)
        self.S.dma("pool", v, src_rows.rearrange("(k p) c -> p k c", p=128), writes=[b])
        return v, b

    def wload_segs(self, src2d, nk, segs):
        t, b = self.wring.get()
        tot = sum(n for _, n in segs)
        v = t[:, 0:nk * tot].rearrange("p (k c) -> p k c", k=nk)
        o = 0
        for (c0, n) in segs:
            self.S.dma("pool", v[:, :, o:o + n], src2d[:, c0:c0 + n].rearrange("(k p) c -> p k c", p=128), writes=[b])
            o += n
        return v, b

    def psum(self):
        return self.pring.get()

    def tmp(self):
        return self.tring.get()

    def build(self):
        nc, S_ = self.nc, self.S
        I = {}
        I["x"] = self.din("x", [S, D])
        I["cst"] = self.din("cst", [128, C_W])
        I["ropeA"] = self.din("ropeA", [128, 2, S])
        I["ropeC"] = self.din("ropeC", [32, 2, S])
        I["ln_in_g"] = self.din("ln_in_g", [D])
        I["ln_in_b"] = self.din("ln_in_b", [D])
        I["w_in"] = self.din("w_in", [DEPTH, D, D_IN])
        I["a_q_norm"] = self.din("a_q_norm", [DEPTH, 64])
        I["a_k_norm"] = self.din("a_k_norm", [DEPTH, 64])
        I["rpbT"] = self.din("rpbT", [DEPTH, 4, 15 * 64, 64])
        I["c_q_norm"] = self.din("c_q_norm", [DEPTH, 192])
        I["c_kv_norm"] = self.din("c_kv_norm", [DEPTH, 128])
        I["c_w_uq"] = self.din("c_w_uq", [DEPTH, 192, 384])
        I["c_w_ukv"] = self.din("c_w_ukv", [DEPTH, 128, 512])
        I["d_conv_w"] = self.din("d_conv_w", [DEPTH, 31, 256])
        I["d_conv_b"] = self.din("d_conv_b", [DEPTH, 256])
        I["d_ln_g"] = self.din("d_ln_g", [DEPTH, 256])
        I["d_ln_b"] = self.din("d_ln_b", [DEPTH, 256])
        I["w_branch"] = self.din("w_branch", [DEPTH, 4, 256, D])
        I["w_out"] = self.din("w_out", [DEPTH, D, D])
        I["ln_mix_g"] = self.din("ln_mix_g", [DEPTH, D])
        I["ln_mix_b"] = self.din("ln_mix_b", [DEPTH, D])
        if not self.light:
          I["ffn_w1"] = self.din("ffn_w1", [1, D, D_FF])
          I["ffn_w3"] = self.din("ffn_w3", [1, D, D_FF])
          I["ffn_w2"] = self.din("ffn_w2", [1, D_FF, D])
          I["moe_router"] = self.din("moe_router", [1, D, NE])
          I["moe_w1"] = self.din("moe_w1", [1, NE, D, D_FFE])
          I["moe_w3"] = self.din("moe_w3", [1, NE, D, D_FFE])
          I["moe_w2"] = self.din("moe_w2", [1, NE, D_FFE, D])
        I["ln_ffn_g"] = self.din("ln_ffn_g", [DEPTH, D])
        I["ln_ffn_b"] = self.din("ln_ffn_b", [DEPTH, D])
        self.I = I
        self.out = nc.dram_tensor("out", [S, D], F32, kind="ExternalOutput").ap()
        self.out_buf = Buf("out")
        self.out_bufs = []

        o = 16384
        self.cst = self.at("cst", [128, C_W], F32, o); o += C_W * 4
        self.bcst = Buf("cst")
        self.vec = self.at("vec", [128, 128], F32, o); o += 512
        self.bvec = Buf("vec")
        self.idb = self.at("idb", [128, 128], BF16, o); o += 256
        self.XH = self.at("XH", [128, KC, S], BF16, o); o += KC * S * 2
        self.XL = self.at("XL", [128, KC, S], BF16, o); o += KC * S * 2
        self.bX = [Buf(f"X{g}") for g in range(NG)]
        self.WOFF = o
        self.wring = self.ring_at("ws", [128, 4096], BF16, o, 4, 8192, phase=False); o += 4 * 8192
        self.wring4 = self.wring
        ntmp = 8
        self.tring = self.ring_at("tmp", [128, 512], F32, o, ntmp, 2048, phase=False); o += ntmp * 2048
        self.sm = self.at("sm", [128, 120], F32, o); o += 480
        self.bsm = Buf("sm")
        self.lnm = self.at("lnm", [128, 512], F32, o); o += 2048
        self.lnv = self.at("lnv", [128, 512], F32, o); o += 2048
        self.blnm, self.blnv = Buf("lnm"), Buf("lnv")
        self.junk = self.at("junk", [128, 8], F32, o); o += 32
        self.bjunk = Buf("junk")
        self.SCR = o
        self.SCR_END = 229376
        banks = []
        for i in range(8):
            banks.append((nc.alloc_psum_tensor(f"ps{i}", [128, 512], F32), Buf(f"ps{i}", excl=True)))
        self.pring = MK.Ring(banks[0:6])
        self.aring = MK.Ring(banks[6:8])
        self.use_spacer = False

        self.ident = self.cst[:, C_ID:C_ID + 128]
        self.ones = self.cst[:, C_ONES:C_ONES + 128]
        self.bd = self.cst[:, C_BD:C_BD + 128]
        self.rm = self.cst[:, C_RM:C_RM + 128]
        self.maskB = self.cst[:, C_MASK:C_MASK + 64]

        S_.dma("sp", self.cst[:], I["cst"], writes=[self.bcst])
        self.cp(self.idb[:], self.bcst, self.ident, [self.bcst])

        self.PH = self.SCR + 32768
        self.barrier()
        self.phase0()
        if self.upto == "p0":
            return self.finish()
        for l in range(DEPTH):
            done = self.layer(l)
            if done:
                break
        return self.finish()

    def finish(self):
        fw = [self.out_buf] + self.out_bufs + list(self.dbg_out.values())
        self.S.emit(final_waits=fw)

    def hilo(self, src, sb, fcs, cols, g):
        hi = self.XH[:, fcs, cols]
        lo = self.XL[:, fcs, cols]
        self.act(hi, self.bX[g], src, [sb], AF.Copy)
        self.tt(lo, self.bX[g], src, hi, [sb, self.bX[g]], ALU.subtract)

    def phase0(self):
        I, S_ = self.I, self.S
        base = self.PH
        gb = self.at("gb", [128, 2, D], F32, base)
        bgb = self.nb("gb")
        S_.dma("sp", gb[:, 0, :], I["ln_in_g"].partition_broadcast(128), writes=[bgb])
        S_.dma("sp", gb[:, 1, :], I["ln_in_b"].partition_broadcast(128), writes=[bgb])
        xin = self.ring_at("xin", [128, D], F32, base + 8192, 2, 4096)
        xn = self.ring_at("xn", [128, D], F32, base + 16384, 2, 4096)
        for tt in range(NT):
            g = tt // 4
            xt, bx = xin.get()
            S_.dma("sp", xt[:], I["x"][tt * 128:(tt + 1) * 128, :], writes=[bx])
            st = self.sm[:, 0:12].rearrange("p (a b) -> p a b", a=2)
            mv = self.sm[:, 12:14]
            for c in range(2):
                S_.op("dve", lambda e, c=c, xt=xt, st=st: e.bn_stats(out=st[:, c, :], in_=xt[:, c * 512:(c + 1) * 512]),
                      reads=[bx], writes=[self.bsm])
            S_.op("dve", lambda e, st=st, mv=mv: e.bn_aggr(out=mv, in_=st), reads=[self.bsm], writes=[self.bsm])
            rs = self.sm[:, 14:15]
            self.act(rs, self.bsm, mv[:, 1:2], [self.bsm], AF.Ln, bias=LN_EPS)
            self.act(rs, self.bsm, rs, [self.bsm], AF.Exp, scale=-0.5)
            xo, bxo = xn.get()
            self.ts(xo[:], bxo, xt[:], mv[:, 0:1], rs, [bx, self.bsm], ALU.subtract, ALU.mult)
            self.tt(xo[:], bxo, xo[:], gb[:, 0, :], [bxo, bgb], ALU.mult)
            self.tt(xo[:], bxo, xo[:], gb[:, 1, :], [bxo, bgb], ALU.add)
            for half in range(2):
                pt, bp = self.psum()
                for j in range(4):
                    fc = half * 4 + j
                    self.tr(pt[:, j * 128:(j + 1) * 128], bp, xo[:, fc * 128:(fc + 1) * 128], self.ident, [bxo, self.bcst])
                self.hilo(pt[:].rearrange("p (a b) -> p a b", a=4), bp, slice(half * 4, half * 4 + 4),
                          slice(tt * 128, (tt + 1) * 128), g)
        self.dump("xh0", self.XH[:], self.bX[3], [128, KC, S], BF16)
        self.dump("xl0", self.XL[:], self.bX[3], [128, KC, S], BF16)

    def proj(self, w, wb, cols, g, m=128):
        pt, bp = self.psum()
        for kc in range(KC):
            self.mm(pt[0:m, :], bp, w[:, kc, cols], self.XH[:, kc, g * 512:(g + 1) * 512], [wb, self.bX[g]],
                    start=(kc == 0), stop=(kc == KC - 1))
        return pt, bp

    def rstd_from(self, ssp, bss, n, eps, m=128):
        r, br = self.tmp()
        self.act(r[0:m, :], br, ssp[0:m, :], [bss], AF.Ln, scale=1.0 / n, bias=eps)
        self.act(r[0:m, :], br, r[0:m, :], [br], AF.Exp, scale=-0.5)
        return r, br

    def rope(self, qn, bqn, m, cos, sin, brope, out, bout=None):
        dsts = out if isinstance(out, list) else [(out, bout, slice(0, m))]
        pr, bpr = self.psum()
        self.mm(pr[0:m, :], bpr, self.rm[0:m, 0:m], qn[0:m, :], [self.bcst, bqn])
        t1, bt1 = self.tmp()
        self.tt(t1[0:m, :], bt1, qn[0:m, :], cos, [bqn, brope], ALU.mult)
        t2, bt2 = self.tmp()
        self.tt(t2[0:m, :], bt2, pr[0:m, :], sin, [bpr, brope], ALU.mult)
        for (o_, b_, ps_) in dsts:
            self.tt(o_, b_, t1[ps_, :], t2[ps_, :], [bt1, bt2], ALU.add)

    def attention(self, nheads, qk_ops, v_ap, bv, scale, br_idx, PT, ytm_ring):
        steps = [(tq, h, kg) for tq in range(NT) for h in range(nheads) for kg in range(4)]

        def emit_qk(st):
            tq, h, kg = st
            ps_, bps = self.psum()
            for j in range(4):
                kt = kg * 4 + j
                ops, rb = qk_ops(h, tq, kt)
                for i, (lh, rh) in enumerate(ops):
                    self.mm(ps_[:, j * 128:(j + 1) * 128], bps, lh, rh, rb, start=(i == 0), stop=(i == len(ops) - 1))
            return ps_, bps

        cur = {}

        def emit_rest(st, ps_, bps):
            tq, h, kg = st
            if h == 0 and kg == 0:
                cur["ytm"], cur["bytm"] = ytm_ring.get()
                cur["po"], cur["bpo"] = self.aring.get()
            po, bpo = cur["po"], cur["bpo"]
            pt_, bpt = PT.get()
            self.act(pt_[:], bpt, ps_[:], [bps], AF.Exp, scale=scale)
            for j in range(4):
                kt = kg * 4 + j
                self.mm(po[:, h * 65:(h + 1) * 65], bpo, pt_[:, j * 128:(j + 1) * 128], v_ap(h, kt), [bpt, bv],
                        start=(kt == 0), stop=(kt == 15))
            if h == nheads - 1 and kg == 3:
                self.norm_out(po, bpo, nheads, cur["ytm"], cur["bytm"], br_idx, tq)

        LOOK = 3
        q = [emit_qk(steps[j]) for j in range(min(LOOK, len(steps)))]
        for i, st in enumerate(steps):
            if i + LOOK < len(steps):
                q.append(emit_qk(steps[i + LOOK]))
            emit_rest(st, *q.pop(0))

    def norm_out(self, po, bpo, nheads, ytm, bytm, br_idx, tq):
        pov = po[:, 0:nheads * 65].rearrange("p (h c) -> p h c", h=nheads)
        rinv = self.sm[:, 16:16 + nheads]
        self.S.op("dve", lambda e: e.reciprocal(out=rinv, in_=pov[:, :, 64]), reads=[bpo], writes=[self.bsm])
        for h in range(nheads):
            self.ts(ytm[:, h * 64:(h + 1) * 64], bytm, pov[:, h, 0:64], rinv[:, h:h + 1], None, [bpo, self.bsm], ALU.mult)
        pt, bp = self.psum()
        for j in range(2):
            self.tr(pt[:, j * 128:(j + 1) * 128], bp, ytm[:, j * 128:(j + 1) * 128], self.ident, [bytm, self.bcst])
        self.act(self.yT[br_idx][:, :, tq * 128:(tq + 1) * 128], self.byT[br_idx],
                 pt[:, 0:256].rearrange("p (a b) -> p a b", a=2), [bp], AF.Copy)

    def ln_fm(self, tT, bt, gc0, bc0, g, final=False):
        s1, bs1 = self.psum()
        for fc in range(KC):
            self.mm(s1[:], bs1, self.ones, tT[:, fc, :], [self.bcst, bt], start=(fc == 0), stop=(fc == KC - 1))
        s2, bs2 = self.psum()
        for fc in range(KC):
            sq, bsq = self.tmp()
            self.act(sq[:], bsq, tT[:, fc, :], [bt], AF.Square)
            self.mm(s2[:], bs2, self.ones, sq[:], [self.bcst, bsq], start=(fc == 0), stop=(fc == KC - 1))
        mean, bmean = self.lnm, self.blnm
        self.act(mean[:], bmean, s1[:], [bs1], AF.Copy, scale=1.0 / D)
        var, bvar = self.lnv, self.blnv
        self.act(var[:], bvar, s1[:], [bs1], AF.Square, scale=1.0 / D)
        self.stt(var[:], bvar, s2[:], 1.0 / D, var[:], [bs2, bvar], ALU.mult, ALU.subtract)
        self.act(var[:], bvar, var[:], [bvar], AF.Ln, bias=LN_EPS)
        self.act(var[:], bvar, var[:], [bvar], AF.Exp, scale=-0.5)
        self.stt(mean[:], bmean, mean[:], -1.0, var[:], [bmean, bvar], ALU.mult, ALU.mult)
        for fc in range(KC):
            u, bu = self.tmp()
            self.tt(u[:], bu, tT[:, fc, :], var[:], [bt, bvar], ALU.mult)
            self.tt(u[:], bu, u[:], mean[:], [bu, bmean], ALU.add)
            gcol, bcol = self.vec[:, gc0 + fc:gc0 + fc + 1], self.vec[:, bc0 + fc:bc0 + fc + 1]
            self.S.op("act", lambda e, u=u, gcol=gcol, bcol=bcol: e.activation(out=u[:], in_=u[:], func=AF.Identity, scale=gcol, bias=bcol),
                      reads=[bu, self.bvec], writes=[bu])
            if not final:
                self.hilo(u[:], bu, fc, slice(g * 512, (g + 1) * 512), g)
            else:
                pt, bp = self.psum()
                for t4 in range(4):
                    self.tr(pt[:, t4 * 128:(t4 + 1) * 128], bp, u[:, t4 * 128:(t4 + 1) * 128], self.ident, [bu, self.bcst])
                og, bog = self.oring.get()
                self.cp(og[:], bog, pt[:].rearrange("p (a b) -> p a b", a=4), [bp], eng="act")
                dst = self.out[g * 512:(g + 1) * 512, fc * 128:(fc + 1) * 128].rearrange("(a p) c -> p a c", p=128)
                self.S.dma("sp", dst, og[:], reads=[bog], track=bog)
                if bog not in self.out_bufs:
                    self.out_bufs.append(bog)

    def layer(self, l):
        if l == 0:
            self.yT = [self.at(f"yT{n}", [128, 2, S], BF16, self.SCR + n * 8192) for n in range(4)]
        self.load_vecs(l)
        steps = [("C", self.branch_C), ("A", self.branch_A), ("B", self.branch_B), ("D", self.branch_D),
                 ("M", self.merge), ("F", self.ffn)]
        self.byT = [Buf(f"yT{n}_{l}") for n in range(4)]
        for b in self.byT:
            b.writer = self.cur_barrier
        for name, fn in steps:
            if name == "F":
                self.phase_bufs.extend(self.byT)
            self.barrier()
            if name in self.skip:
                continue
            fn(l)
            if self.upto.startswith(f"L{l}{name}"):
                return True
        self.barrier()
        return False

    V_AQ, V_AK, V_CQ, V_CKV, V_DCB, V_DLG, V_DLB, V_LMG, V_LMB, V_LFG, V_LFB = 0, 1, 2, 4, 5, 7, 9, 11, 19, 27, 35

    def load_vecs(self, l):
        I, S_, v, b = self.I, self.S, self.vec, self.bvec

        def col(ap1d):
            return ap1d.rearrange("(p o) -> p o", o=1)
        for half in range(2):
            S_.dma("sp", v[half * 64:(half + 1) * 64, self.V_AQ:self.V_AQ + 1], col(I["a_q_norm"][l]), writes=[b])
            S_.dma("sp", v[half * 64:(half + 1) * 64, self.V_AK:self.V_AK + 1], col(I["a_k_norm"][l]), writes=[b])
        S_.dma("sp", v[:, self.V_CQ:self.V_CQ + 1], col(I["c_q_norm"][l][0:128]), writes=[b])
        S_.dma("sp", v[0:64, self.V_CQ + 1:self.V_CQ + 2], col(I["c_q_norm"][l][128:192]), writes=[b])
        S_.dma("sp", v[:, self.V_CKV:self.V_CKV + 1], col(I["c_kv_norm"][l]), writes=[b])
        for nm, c0, n in (("d_conv_b", self.V_DCB, 2), ("d_ln_g", self.V_DLG, 2), ("d_ln_b", self.V_DLB, 2),
                          ("ln_mix_g", self.V_LMG, 8), ("ln_mix_b", self.V_LMB, 8), ("ln_ffn_g", self.V_LFG, 8),
                          ("ln_ffn_b", self.V_LFB, 8)):
            for c in range(n):
                S_.dma("sp", v[:, c0 + c:c0 + c + 1], col(I[nm][l][c * 128:(c + 1) * 128]), writes=[b])

    def branch_A(self, l):
        I, S_ = self.I, self.S
        base = self.PH
        QA = [self.at(f"QA{h}", [128, S], BF16, base + h * 4096) for h in range(4)]
        KA = self.at("KA", [128, S], BF16, base + 16384)
        VA = self.at("VA", [128, NT, 2, 65], BF16, base + 20480)
        bQ, bK, bV = [self.nb(f"QA{h}") for h in range(4)], self.nb("KA"), self.nb("VA")
        o = base + 20480 + 4160
        for h in range(4):
            S_.op("dve", lambda e, h=h: e.memset(QA[h][:], 0.0), writes=[bQ[h]])
        rope = self.ring_at("ropeA", [128, 2, 512], F32, o, 2, 4096); o += 8192
        PT = self.ring_at("PTa", [128, 512], BF16, o, 4, 1024); o += 4096
        ytm = self.ring_at("ytmA", [128, 256], F32, o, 2, 1024); o += 2048
        assert o <= self.SCR_END
        S_.op("dve", lambda e: e.memset(VA[:, :, :, 64:65], 1.0), writes=[bV])
        gq = self.vec[:, self.V_AQ:self.V_AQ + 1]
        gk = self.vec[:, self.V_AK:self.V_AK + 1]
        for g in range(NG):
            w, wb = self.wload_segs(I["w_in"][l], KC, [(0, 64), (128, 64), (64, 64), (192, 64), (256, 256)])
            rt, brt = rope.get()
            S_.dma("sp", rt[:], I["ropeA"][:, :, g * 512:(g + 1) * 512], writes=[brt])
            for c in range(3):
                if c < 2:
                    pt, bp = self.proj(w, wb, slice(c * 128, (c + 1) * 128), g)
                    gcol = gq
                    dsts = [(QA[c][0:64, g * 512:(g + 1) * 512], bQ[c], slice(0, 64)),
                            (QA[c + 2][64:128, g * 512:(g + 1) * 512], bQ[c + 2], slice(64, 128))]
                else:
                    pt, bp = self.proj(w, wb, slice(256, 384), g)
                    gcol = gk
                    dsts = [(KA[:, g * 512:(g + 1) * 512], bK, slice(0, 128))]
                sq, bsq = self.tmp()
                self.act(sq[:], bsq, pt[:], [bp], AF.Square)
                qf, bqf = self.tmp()
                self.cp(qf[:], bqf, pt[:], [bp], eng="act")
                pss, bpss = self.psum()
                self.mm(pss[:], bpss, self.bd, sq[:], [self.bcst, bsq])
                r, br = self.rstd_from(pss, bpss, 64.0, RMS_EPS)
                qn, bqn = self.tmp()
                self.stt(qn[:], bqn, qf[:], gcol, r[:], [bqf, self.bvec, br], ALU.mult, ALU.mult)
                self.rope(qn, bqn, 128, rt[:, 0, :], rt[:, 1, :], brt, dsts)
            for t4 in range(4):
                tt = g * 4 + t4
                pv, bpv = self.psum()
                for kc in range(KC):
                    self.mm(pv[:, 0:128], bpv, self.XH[:, kc, tt * 128:(tt + 1) * 128], w[:, kc, 384:512], [wb, self.bX[g]],
                            start=(kc == 0), stop=(kc == KC - 1))
                self.cp(VA[:, tt, :, 0:64], bV, pv[:, 0:128].rearrange("p (a d) -> p a d", a=2), [bpv])
        self.dump("qa0", QA[0][:], bQ[0], [128, S], BF16)
        self.dump("ka", KA[:], bK, [128, S], BF16)
        if self.upto.endswith("A1"):
            return

        def qk_ops(h, tq, kt):
            return [(KA[:, kt * 128:(kt + 1) * 128], QA[h][:, tq * 128:(tq + 1) * 128])], [bK, bQ[h]]

        def v_ap(h, kt):
            return VA[:, kt, h // 2, :]
        self.attention(4, qk_ops, v_ap, bV, 0.125, 0, PT, ytm)
        self.dump("yTa", self.yT[0][:], self.byT[0], [128, 2, S], BF16)

    def branch_C(self, l):
        I, S_ = self.I, self.S
        o = self.PH
        QC = [self.at(f"QC{h}", [128, S], BF16, o + h * 4096) for h in range(4)]; o += 16384
        KC_ = [self.at(f"KC{h}", [128, S], BF16, o + h * 4096) for h in range(4)]; o += 16384
        VC = self.at("VC", [128, NT, 4, 65], BF16, o); o += 8320
        rope = self.ring_at("ropeC", [32, 2, 512], F32, o, 1, 4096); o += 4096
        PT = self.ring_at("PTc", [128, 512], BF16, o, 4, 1024); o += 4096
        ytm = self.ring_at("ytmC", [128, 256], F32, o, 2, 1024); o += 2048
        cqn = self.at("cqn", [128, 2, 512], BF16, o); o += 2048
        ckvn = self.at("ckvn", [128, 512], BF16, o); o += 1024
        wuq = self.at("wuq", [128, 2, 384], BF16, o); o += 1536
        wukv = self.at("wukv", [128, 1, 512], BF16, o); o += 1024
        assert o <= self.SCR_END, o
        bQ = [self.nb(f"QC{h}") for h in range(4)]
        bK = [self.nb(f"KC{h}") for h in range(4)]
        bV, bcqn, bckvn, bwuq, bwukv = self.nb("VC"), self.nb("cqn"), self.nb("ckvn"), self.nb("wuq"), self.nb("wukv")
        S_.op("dve", lambda e: e.memset(VC[:, :, :, 64:65], 1.0), writes=[bV])
        wpre = self.wload(I["w_in"][l][:, 1280:1632], KC, 352)
        raw, braw = self.wring.get()
        ruq = raw[:, 0:768].rearrange("p (k c) -> p k c", k=2)
        rukv = raw[:, 768:1280]
        S_.dma("pool", ruq[:, 0, :], I["c_w_uq"][l][0:128, :], writes=[braw])
        S_.dma("pool", ruq[0:64, 1, :], I["c_w_uq"][l][128:192, :], writes=[braw])
        S_.dma("pool", rukv, I["c_w_ukv"][l], writes=[braw])
        for k in range(2):
            pp = slice(0, 128) if k == 0 else slice(0, 64)
            rv = ruq[pp, k, :].rearrange("p (h c) -> p h c", h=4)
            self.cp(wuq[pp, k, 0:256].rearrange("p (h c) -> p h c", h=4), bwuq, rv[:, :, 0:64], [braw])
            self.cp(wuq[pp, k, 256:384].rearrange("p (h c) -> p h c", h=4), bwuq, rv[:, :, 64:96], [braw], eng="act")
        rkv = rukv.rearrange("p (h c) -> p h c", h=4)
        self.cp(wukv[:, 0, 0:256].rearrange("p (h c) -> p h c", h=4), bwukv, rkv[:, :, 0:64], [braw])
        self.cp(wukv[:, 0, 256:512].rearrange("p (h c) -> p h c", h=4), bwukv, rkv[:, :, 64:128], [braw], eng="act")
        gcq = self.vec[:, self.V_CQ:self.V_CQ + 2]
        gckv = self.vec[:, self.V_CKV:self.V_CKV + 1]
        for g in range(NG):
            gs = slice(g * 512, (g + 1) * 512)
            w, wb = wpre if g == 0 else self.wload(I["w_in"][l][:, 1280:1632], KC, 352)
            rt, brt = rope.get()
            S_.dma("sp", rt[:], I["ropeC"][:, :, gs], writes=[brt])
            p0, bp0 = self.proj(w, wb, slice(0, 128), g)
            p1, bp1 = self.proj(w, wb, slice(128, 192), g, m=64)
            sq0, bs0 = self.tmp()
            sq1, bs1 = self.tmp()
            self.act(sq0[:], bs0, p0[:], [bp0], AF.Square)
            self.act(sq1[0:64, :], bs1, p1[0:64, :], [bp1], AF.Square)
            pss, bpss = self.psum()
            self.mm(pss[:], bpss, self.ones, sq0[:], [self.bcst, bs0], start=True, stop=False)
            self.mm(pss[:], bpss, self.ones[0:64, :], sq1[0:64, :], [self.bcst, bs1], start=False, stop=True)
            r, br = self.rstd_from(pss, bpss, 192.0, RMS_EPS)
            self.stt(cqn[:, 0, :], bcqn, p0[:], gcq[:, 0:1], r[:], [bp0, self.bvec, br], ALU.mult, ALU.mult)
            self.stt(cqn[0:64, 1, :], bcqn, p1[0:64, :], gcq[0:64, 1:2], r[0:64, :], [bp1, self.bvec, br], ALU.mult, ALU.mult)
            p2, bp2 = self.proj(w, wb, slice(192, 320), g)
            sq2, bs2 = self.tmp()
            self.act(sq2[:], bs2, p2[:], [bp2], AF.Square)
            pss2, bpss2 = self.psum()
            self.mm(pss2[:], bpss2, self.ones, sq2[:], [self.bcst, bs2])
            r2, br2 = self.rstd_from(pss2, bpss2, 128.0, RMS_EPS)
            self.stt(ckvn[:], bckvn, p2[:], gckv, r2[:], [bp2, self.bvec, br2], ALU.mult, ALU.mult)
            p3, bp3 = self.proj(w, wb, slice(320, 352), g, m=32)
            kf, bkf = self.tmp()
            self.cp(kf[0:32, :], bkf, p3[0:32, :], [bp3])
            kr, bkr = self.tmp()
            self.rope(kf, bkf, 32, rt[:, 0, :], rt[:, 1, :], brt, kr[0:32, :], bkr)
            for h in range(4):
                self.cp(KC_[h][64:96, gs], bK[h], kr[0:32, :], [bkr], eng=("act" if h % 2 else "dve"))
            for c in range(2):
                pq, bpq = self.psum()
                self.mm(pq[:], bpq, wuq[:, 0, c * 128:(c + 1) * 128], cqn[:, 0, :], [bwuq, bcqn], start=True, stop=False)
                self.mm(pq[:], bpq, wuq[0:64, 1, c * 128:(c + 1) * 128], cqn[0:64, 1, :], [bwuq, bcqn], start=False, stop=True)
                self.cp(QC[2 * c][0:64, gs], bQ[2 * c], pq[0:64, :], [bpq], eng="act")
                self.cp(QC[2 * c + 1][0:64, gs], bQ[2 * c + 1], pq[64:128, :], [bpq])
            for h in range(4):
                pq, bpq = self.psum()
                self.mm(pq[0:32, :], bpq, wuq[:, 0, 256 + h * 32:256 + (h + 1) * 32], cqn[:, 0, :], [bwuq, bcqn], start=True, stop=False)
                self.mm(pq[0:32, :], bpq, wuq[0:64, 1, 256 + h * 32:256 + (h + 1) * 32], cqn[0:64, 1, :], [bwuq, bcqn], start=False, stop=True)
                qf, bqf = self.tmp()
                self.cp(qf[0:32, :], bqf, pq[0:32, :], [bpq])
                self.rope(qf, bqf, 32, rt[:, 0, :], rt[:, 1, :], brt, QC[h][64:96, gs], bQ[h])
            for c in range(2):
                pk, bpk = self.psum()
                self.mm(pk[:], bpk, wukv[:, 0, c * 128:(c + 1) * 128], ckvn[:], [bwukv, bckvn])
                self.cp(KC_[2 * c][0:64, gs], bK[2 * c], pk[0:64, :], [bpk], eng="act")
                self.cp(KC_[2 * c + 1][0:64, gs], bK[2 * c + 1], pk[64:128, :], [bpk])
            for t4 in range(4):
                tt = g * 4 + t4
                pv, bpv = self.psum()
                self.mm(pv[:, 0:256], bpv, ckvn[:, t4 * 128:(t4 + 1) * 128], wukv[:, 0, 256:512], [bckvn, bwukv])
                self.cp(VC[:, tt, :, 0:64], bV, pv[:, 0:256].rearrange("p (a d) -> p a d", a=4), [bpv])
        self.dump("qc0", QC[0][:], bQ[0], [128, S], BF16)
        self.dump("kc1", KC_[1][:], bK[1], [128, S], BF16)
        if self.upto.endswith("C1"):
            return

        def qk_ops(h, tq, kt):
            return ([(KC_[h][0:96, kt * 128:(kt + 1) * 128], QC[h][0:96, tq * 128:(tq + 1) * 128])], [bK[h], bQ[h]])

        def v_ap(h, kt):
            return VC[:, kt, h, :]
        self.attention(4, qk_ops, v_ap, bV, 96.0 ** -0.5, 2, PT, ytm)
        self.dump("yTc", self.yT[2][:], self.byT[2], [128, 2, S], BF16)

    def branch_B(self, l):
        I, S_ = self.I, self.S
        o = self.PH
        QB = [self.at(f"QB{h}", [128, S], BF16, o + h * 4096) for h in range(4)]; o += 16384
        KB = [self.at(f"KB{c}", [128, S], BF16, o + c * 4096) for c in range(2)]; o += 8192
        VB = self.at("VB", [128, NT, 4, 65], BF16, o); o += 8320
        VBs = self.at("VBs", [128, NT - 1, 4, 65], BF16, o); o += 7808
        tbl = self.at("tblB", [128, 4, 14, 64], BF16, o); o += 7168
        PT = self.ring_at("PTb", [128, 4, 4, 64], BF16, o, 3, 2048); o += 6144
        ytm = self.ring_at("ytmB", [128, 256], F32, o, 2, 1024); o += 2048
        assert o <= self.SCR_END, o
        bQ, bK = [self.nb(f"QB{h}") for h in range(4)], [self.nb("KB0"), self.nb("KB1")]
        for h in range(4):
            S_.op("dve", lambda e, h=h: e.memset(QB[h][:], 0.0), writes=[bQ[h]])
        bV, bVs, btbl = self.nb("VB"), self.nb("VBs"), self.nb("tblB")
        S_.op("dve", lambda e: e.memset(VB[:, :, :, 64:65], 1.0), writes=[bV])
        S_.op("dve", lambda e: e.memset(VBs[:, :, :, 64:65], 1.0), writes=[bVs])
        for h in range(4):
            for p7 in range(2):
                tf, btf = self.tmp()
                tfv = tf[:, 0:448].rearrange("p (a b) -> p a b", a=7)
                for pj in range(7):
                    pi = p7 * 7 + pj
                    S_.dma("sp", tfv[:, pj, :], I["rpbT"][l, h, pi * 64:pi * 64 + 128, :], writes=[btf])
                for pj in range(7):
                    pi = p7 * 7 + pj
                    self.tt(tbl[:, h, pi, :], btbl, tfv[:, pj, :], self.maskB, [btf, self.bcst], ALU.add)
        for g in range(NG):
            gs = slice(g * 512, (g + 1) * 512)
            w, wb = self.wload(I["w_in"][l][:, 512:1024], KC, 512)
            wv, wvb = self.wload(I["w_in"][l][:, 1024:1280], KC, 256)
            for c in range(2):
                pq, bpq = self.proj(w, wb, slice(c * 128, (c + 1) * 128), g)
                for h2 in range(2):
                    self.cp(QB[2 * c + h2][h2 * 64:(h2 + 1) * 64, gs], bQ[2 * c + h2], pq[h2 * 64:(h2 + 1) * 64, :], [bpq],
                            eng="act", scale=0.125)
                pk, bpk = self.proj(w, wb, slice(256 + c * 128, 256 + (c + 1) * 128), g)
                self.cp(KB[c][:, gs], bK[c], pk[:], [bpk])
            for t4 in range(4):
                tt = g * 4 + t4
                for sh in range(2):
                    if sh == 1 and tt == NT - 1:
                        continue
                    t0 = tt * 128 + sh * 64
                    rb = [wvb, self.bX[g]] + ([self.bX[g + 1]] if (sh == 1 and t4 == 3) else [])
                    pv, bpv = self.psum()
                    for kc in range(KC):
                        self.mm(pv[:, 0:256], bpv, self.XH[:, kc, t0:t0 + 128], wv[:, kc, :], rb,
                                start=(kc == 0), stop=(kc == KC - 1))
                    dstV, bdst = (VB, bV) if sh == 0 else (VBs, bVs)
                    self.cp(dstV[:, tt, :, 0:64], bdst, pv[:, 0:256].rearrange("p (a d) -> p a d", a=4), [bpv],
                            eng=("act" if sh else "dve"))
        rows = list(range(2 * NT))

        def rs_of(r):
            return min(max(r - 4, 0), 24)

        def emit_qk(r):
            rs = rs_of(r)
            k0 = rs * 64
            out = []
            for hp in range(2):
                ps_, bps = self.psum()
                for h2 in range(2):
                    h = hp * 2 + h2
                    for j in range(4):
                        reg = ps_[:, h2 * 256 + j * 64:h2 * 256 + (j + 1) * 64]
                        self.mm(reg, bps, KB[hp][:, k0 + 128 * j:k0 + 128 * (j + 1)], QB[h][:, r * 64:(r + 1) * 64],
                                [bK[hp], bQ[h]])
                out.append((ps_, bps))
            return out

        cur = {}

        def emit_rest(r, banks):
            tq, rr = r // 2, r % 2
            rs = rs_of(r)
            if rr == 0:
                cur["ytm"], cur["bytm"] = ytm.get()
                cur["po"], cur["bpo"] = self.aring.get()
            po, bpo = cur["po"], cur["bpo"]
            pt_, bpt = PT.get()
            for hp in range(2):
                ps_, bps = banks[hp]
                sc, bsc = self.tmp()
                pi0 = rs - r + 7
                for h2 in range(2):
                    h = hp * 2 + h2
                    self.tt(sc[:, h2 * 256:(h2 + 1) * 256].rearrange("p (a b) -> p a b", a=4),
                            bsc, ps_[:, h2 * 256:(h2 + 1) * 256].rearrange("p (a b) -> p a b", a=4),
                            tbl[:, h, pi0:pi0 + 7:2, :], [bps, btbl], ALU.add)
                self.act(pt_[:, hp * 2:hp * 2 + 2, :, :], bpt, sc[:].rearrange("p (a b c) -> p a b c", a=2, b=4), [bsc], AF.Exp)
            for h in range(4):
                for j in range(4):
                    if rs % 2 == 0:
                        vt, bvt = VB[:, rs // 2 + j, h, :], bV
                    else:
                        vt, bvt = VBs[:, (rs - 1) // 2 + j, h, :], bVs
                    self.mm(po[rr * 64:(rr + 1) * 64, h * 65:(h + 1) * 65], bpo, pt_[:, h, j, :], vt, [bpt, bvt],
                            start=(j == 0), stop=(j == 3))
            if rr == 1:
                self.norm_out(po, bpo, 4, cur["ytm"], cur["bytm"], 1, tq)

        nxt = emit_qk(rows[0])
        for i, r in enumerate(rows):
            this = nxt
            nxt = emit_qk(rows[i + 1]) if i + 1 < len(rows) else None
            emit_rest(r, this)
        self.dump("yTb", self.yT[1][:], self.byT[1], [128, 2, S], BF16)

    def branch_D(self, l):
        I, S_ = self.I, self.S
        o = self.PH
        uT = self.at("uT", [128, 2, S + 30], BF16, o); o += 2 * (S + 30) * 2 + 8
        Dg = self.at("Dg", [128, 31, 128], BF16, o); o += 31 * 128 * 2
        vT = self.at("vT", [128, 2, S], F32, o); o += 2 * S * 4
        cw = self.at("cw", [128, 256], F32, o); o += 1024
        wc = self.at("wc", [128, 2, 32], F32, o); o += 256
        assert o <= self.SCR_END, o
        buT, bDg, bvT, bcw, bwc = self.nb("uT"), self.nb("Dg"), self.nb("vT"), self.nb("cw"), self.nb("wc")
        S_.op("dve", lambda e: e.memset(uT[:, :, 0:15], 0.0), writes=[buT])
        S_.op("dve", lambda e: e.memset(uT[:, :, S + 15:S + 30], 0.0), writes=[buT])
        S_.dma("sp", cw[0:31, :], I["d_conv_w"][l], writes=[bcw])
        pt, bp = self.psum()
        for ct in range(2):
            self.tr(pt[:, ct * 32:ct * 32 + 31], bp, cw[0:31, ct * 128:(ct + 1) * 128], self.ident[0:31, 0:31], [bcw, self.bcst])
        self.cp(wc[:, :, 0:31], bwc, pt[:, 0:64].rearrange("p (a b) -> p a b", a=2)[:, :, 0:31], [bp])
        for g in range(NG):
            w, wb = self.wload(I["w_in"][l][:, 1632:2144], KC, 512)
            for ct in range(2):
                pa, bpa = self.proj(w, wb, slice(ct * 128, (ct + 1) * 128), g)
                pb, bpb = self.proj(w, wb, slice(256 + ct * 128, 256 + (ct + 1) * 128), g)
                sg, bsg = self.tmp()
                self.act(sg[:], bsg, pb[:], [bpb], AF.Sigmoid)
                self.tt(uT[:, ct, 15 + g * 512:15 + (g + 1) * 512], buT, pa[:], sg[:], [bpa, bsg], ALU.mult)
        for ct in range(2):
            for j in range(31):
                self.ts(Dg[:, j, :], bDg, self.idb[:], wc[:, ct, j:j + 1], None, [self.bcst, bwc], ALU.mult)
            for g in range(NG):
                pc, bpc = self.psum()
                for j in range(31):
                    self.mm(pc[:], bpc, Dg[:, j, :], uT[:, ct, g * 512 + j:g * 512 + j + 512], [bDg, buT],
                            start=(j == 0), stop=(j == 30))
                self.act(vT[:, ct, g * 512:(g + 1) * 512], bvT, pc[:], [bpc, self.bvec], AF.Identity,
                         bias=self.vec[:, self.V_DCB + ct:self.V_DCB + ct + 1])
        for g in range(NG):
            gs = slice(g * 512, (g + 1) * 512)
            s1, bs1 = self.psum()
            s2, bs2 = self.psum()
            for ct in range(2):
                self.mm(s1[:], bs1, self.ones, vT[:, ct, gs], [self.bcst, bvT], start=(ct == 0), stop=(ct == 1))
            for ct in range(2):
                sq, bsq = self.tmp()
                self.act(sq[:], bsq, vT[:, ct, gs], [bvT], AF.Square)
                self.mm(s2[:], bs2, self.ones, sq[:], [self.bcst, bsq], start=(ct == 0), stop=(ct == 1))
            mean, bmean = self.lnm, self.blnm
            self.act(mean[:], bmean, s1[:], [bs1], AF.Copy, scale=1.0 / 256)
            var, bvar = self.lnv, self.blnv
            self.act(var[:], bvar, s1[:], [bs1], AF.Square, scale=1.0 / 256)
            self.stt(var[:], bvar, s2[:], 1.0 / 256, var[:], [bs2, bvar], ALU.mult, ALU.subtract)
            self.act(var[:], bvar, var[:], [bvar], AF.Ln, bias=LN_EPS)
            self.act(var[:], bvar, var[:], [bvar], AF.Exp, scale=-0.5)
            self.stt(mean[:], bmean, mean[:], -1.0, var[:], [bmean, bvar], ALU.mult, ALU.mult)
            for ct in range(2):
                u, bu = self.tmp()
                self.tt(u[:], bu, vT[:, ct, gs], var[:], [bvT, bvar], ALU.mult)
                self.tt(u[:], bu, u[:], mean[:], [bu, bmean], ALU.add)
                yT3 = self.yT[3]
                self.S.op("act", lambda e, u=u, ct=ct, gs=gs, yT3=yT3: e.activation(
                    out=yT3[:, ct, gs], in_=u[:], func=AF.Silu,
                    scale=self.vec[:, self.V_DLG + ct:self.V_DLG + ct + 1],
                    bias=self.vec[:, self.V_DLB + ct:self.V_DLB + ct + 1]), reads=[bu, self.bvec], writes=[self.byT[3]])
        self.dump("yTd", self.yT[3][:], self.byT[3], [128, 2, S], BF16)

    def merge(self, l):
        I, S_ = self.I, self.S
        o = self.PH
        merged = self.at("merged", [128, KC, 1024], BF16, o); o += 16384
        macc = self.at("macc", [128, 8, 512], F32, o); o += 16384
        tT = self.at("tT", [128, KC, 512], F32, o); o += 16384
        assert o <= self.SCR_END, o
        bmer, bmacc, btT = self.nb("merged"), [self.nb(f"macc{i}") for i in range(8)], self.nb("tT")
        for th in range(2):
            for ocq in range(2):
                for n in range(4):
                    c0 = 2144 + n * 1024 + ocq * 512
                    wg, bwg = self.wload(I["w_in"][l][:, c0:c0 + 512], KC, 512)
                    wbr, bwbr = self.wload(I["w_branch"][l, n][:, ocq * 512:(ocq + 1) * 512], 2, 512)
                    for oci in range(4):
                        for gi in range(2):
                            g = 2 * th + gi
                            idx = oci * 2 + gi
                            oc = ocq * 4 + oci
                            pg, bpg = self.proj(wg, bwg, slice(oci * 128, (oci + 1) * 128), g)
                            pb, bpb = self.psum()
                            for k2 in range(2):
                                self.mm(pb[:], bpb, wbr[:, k2, oci * 128:(oci + 1) * 128], self.yT[n][:, k2, g * 512:(g + 1) * 512],
                                        [bwbr, self.byT[n]], start=(k2 == 0), stop=(k2 == 1))
                            sg, bsg = self.tmp()
                            self.act(sg[:], bsg, pg[:], [bpg], AF.Sigmoid)
                            if n == 0:
                                self.tt(macc[:, idx, :], bmacc[idx], pb[:], sg[:], [bpb, bsg], ALU.mult)
                            else:
                                self.tt(sg[:], bsg, pb[:], sg[:], [bpb, bsg], ALU.mult)
                                if n < 3:
                                    self.tt(macc[:, idx, :], bmacc[idx], macc[:, idx, :], sg[:], [bmacc[idx], bsg], ALU.add)
                                else:
                                    self.tt(merged[:, oc, gi * 512:(gi + 1) * 512], bmer, macc[:, idx, :], sg[:], [bmacc[idx], bsg],
                                            ALU.add)
            if th == 0:
                self.dump("merged0", merged[:], bmer, [128, KC, 1024], BF16)
            for gi in range(2):
                g = 2 * th + gi
                gs = slice(g * 512, (g + 1) * 512)
                for half in range(2):
                    wo, bwo = self.wload(I["w_out"][l][:, half * 512:(half + 1) * 512], KC, 512)
                    for oi in range(4):
                        oc2 = half * 4 + oi
                        po, bpo = self.psum()
                        for fc in range(KC):
                            self.mm(po[:], bpo, wo[:, fc, oi * 128:(oi + 1) * 128], merged[:, fc, gi * 512:(gi + 1) * 512], [bwo, bmer],
                                    start=(fc == 0), stop=(fc == KC - 1))
                        self.stt(tT[:, oc2, :], btT, self.XH[:, oc2, gs], ALPHA, po[:], [self.bX[g], bpo], ALU.mult, ALU.add)
                        self.stt(tT[:, oc2, :], btT, self.XL[:, oc2, gs], ALPHA, tT[:, oc2, :], [self.bX[g], btT], ALU.mult, ALU.add)
                if g == 0:
                    self.dump("tT0", tT[:], btT, [128, KC, 512])
                self.ln_fm(tT, btT, self.V_LMG, self.V_LMB, g)
        self.dump(f"x1h{l}", self.XH[:], self.bX[3], [128, KC, S], BF16)
        self.dump(f"x1l{l}", self.XL[:], self.bX[3], [128, KC, S], BF16)

    def ffn(self, l):
        I, S_ = self.I, self.S
        moe = (l % 2 == 1)
        final = (l == DEPTH - 1)
        o = self.SCR
        acc = self.at("acc", [128, KC, 1024], F32, o); o += 32768
        hring = self.ring_at("hT", [128, 4, 1024], BF16, o, 2, 8192); o += 16384
        gBr = self.ring_at("gB", [128, 1024], F32, o, 2, 4096); o += 8192
        self.oring = self.ring_at("ostg", [128, 4, 128], F32, o, 2, 2048); o += 4096
        gtm = self.at("gtm", [128, NT, 8], F32, o); o += 512
        rt32 = self.at("rt32", [128, KC, 8], F32, o); o += 256
        rth = self.at("rth", [128, KC, 8], BF16, o); o += 128
        rtl = self.at("rtl", [128, KC, 8], BF16, o); o += 128
        lg = self.at("lg", [128, 16], F32, o); o += 64
        nextra = (self.SCR_END - o) // 8192
        extra = [(self.at("wsx", [128, 4096], BF16, o + i * 8192), self.nb(f"wsx{i}")) for i in range(nextra)]
        self.wring = MK.Ring(self.wring4.items + extra)
        bacc, bgtm, brt, blg = self.nb("acc"), self.nb("gtm"), self.nb("rt"), self.nb("lg")
        if moe:
            e0 = l // 2
            S_.dma("sp", rt32[:], I["moe_router"][e0].rearrange("(k p) e -> p k e", p=128), writes=[brt])
            self.cp(rth[:], brt, rt32[:], [brt])
            self.tt(rtl[:], brt, rt32[:], rth[:], [brt], ALU.subtract)
            for tt in range(NT):
                g = tt // 4
                ts_ = slice(tt * 128, (tt + 1) * 128)
                pl, bpl = self.psum()
                n = 0
                for kc in range(KC):
                    for (xa, ra) in ((self.XH, rth), (self.XH, rtl), (self.XL, rth)):
                        self.mm(pl[:, 0:8], bpl, xa[:, kc, ts_], ra[:, kc, :], [self.bX[g], brt], start=(n == 0), stop=(n == 3 * KC - 1))
                        n += 1
                self.cp(lg[:, 0:8], blg, pl[:, 0:8], [bpl])
                if tt == 0:
                    self.dump("lg0", lg[:, 0:8], blg, [128, 8])
                S_.op("dve", lambda e: e.max(out=lg[:, 8:16], in_=lg[:, 0:8]), reads=[blg], writes=[blg])
                sm = self.sm
                self.tt(sm[:, 32:33], self.bsm, lg[:, 9:10], lg[:, 8:9], [blg], ALU.subtract)
                self.act(sm[:, 33:34], self.bsm, sm[:, 32:33], [self.bsm], AF.Exp)
                self.ts(sm[:, 34:35], self.bsm, sm[:, 33:34], 1.0, None, [self.bsm], ALU.add)
                S_.op("dve", lambda e: e.reciprocal(out=sm[:, 35:36], in_=sm[:, 34:35]), reads=[self.bsm], writes=[self.bsm])
                self.tt(sm[:, 36:37], self.bsm, sm[:, 33:34], sm[:, 35:36], [self.bsm], ALU.mult)
                self.ts(gtm[:, tt, :], bgtm, lg[:, 0:8], lg[:, 8:9], sm[:, 35:36], [blg, self.bsm], ALU.is_equal, ALU.mult)
                self.ts(sm[:, 40:48], self.bsm, lg[:, 0:8], lg[:, 9:10], sm[:, 36:37], [blg, self.bsm], ALU.is_equal, ALU.mult)
                self.tt(gtm[:, tt, :], bgtm, gtm[:, tt, :], sm[:, 40:48], [bgtm, self.bsm], ALU.add)
            self.dump("gtm", gtm[:], bgtm, [128, NT, 8])
            w1a, w3a, w2a = I["moe_w1"][e0], I["moe_w3"][e0], I["moe_w2"][e0]
            nff, experts = D_FFE // 128, list(range(NE))
        else:
            e0 = l // 2
            w1a, w3a, w2a = I["ffn_w1"], I["ffn_w3"], I["ffn_w2"]
            nff, experts = D_FF // 128, [e0]
        blocks = [(f0, min(4, nff - f0)) for f0 in range(0, nff, 4)]
        for th in range(2):
            work = [(e, f0, nf) for e in experts for (f0, nf) in blocks]
            gBs = {}

            def h_phase(item):
                e, f0, nf = item
                if moe and e not in gBs:
                    gB, bgB = gBr.get()
                    for t8 in range(8):
                        tt = th * 8 + t8
                        if t8 % 4 == 0:
                            pgb, bpgb = self.psum()
                        rep, brep = self.tmp()
                        self.ts(rep[:, 0:128], brep, self.ones, gtm[:, tt, e:e + 1], None, [self.bcst, bgtm], ALU.mult)
                        self.mm(pgb[:, (t8 % 4) * 128:(t8 % 4 + 1) * 128], bpgb, rep[:, 0:128], self.ident, [brep, self.bcst])
                        if t8 % 4 == 3:
                            self.cp(gB[:, (t8 // 4) * 512:(t8 // 4 + 1) * 512], bgB, pgb[:], [bpgb], eng="act")
                    gBs[e] = (gB, bgB)
                w1s, bw1 = self.wload(w1a[e][:, f0 * 128:(f0 + nf) * 128], KC, nf * 128)
                w3s, bw3 = self.wload(w3a[e][:, f0 * 128:(f0 + nf) * 128], KC, nf * 128)
                w2s, bw2 = self.wload(w2a[e][f0 * 128:(f0 + nf) * 128, :], nf, 1024)
                hT, bh = hring.get()
                for f in range(nf):
                    for gi in range(2):
                        g = 2 * th + gi
                        p1, bp1 = self.proj(w1s, bw1, slice(f * 128, (f + 1) * 128), g)
                        p3, bp3 = self.proj(w3s, bw3, slice(f * 128, (f + 1) * 128), g)
                        s_, bs_ = self.tmp()
                        self.act(s_[:], bs_, p1[:], [bp1], AF.Silu)
                        if moe:
                            gB, bgB = gBs[e]
                            self.tt(s_[:], bs_, s_[:], gB[:, gi * 512:(gi + 1) * 512], [bs_, bgB], ALU.mult)
                        self.tt(hT[:, f, gi * 512:(gi + 1) * 512], bh, p3[:], s_[:], [bp3, bs_], ALU.mult)
                return (nf, w2s, bw2, hT, bh)

            def w2_phase(st, first):
                nf, w2s, bw2, hT, bh = st
                for oc in range(KC):
                    for gi in range(2):
                        g = 2 * th + gi
                        gs = slice(g * 512, (g + 1) * 512)
                        po, bpo = self.psum()
                        for f in range(nf):
                            self.mm(po[:], bpo, w2s[:, f, oc * 128:(oc + 1) * 128], hT[:, f, gi * 512:(gi + 1) * 512], [bw2, bh],
                                    start=(f == 0), stop=(f == nf - 1))
                        a_ = acc[:, oc, gi * 512:(gi + 1) * 512]
                        if first:
                            self.stt(a_, bacc, self.XH[:, oc, gs], ALPHA, po[:], [self.bX[g], bpo], ALU.mult, ALU.add)
                            self.stt(a_, bacc, self.XL[:, oc, gs], ALPHA, a_, [self.bX[g], bacc], ALU.mult, ALU.add)
                        else:
                            self.tt(a_, bacc, po[:], a_, [bpo, bacc], ALU.add)

            prev = h_phase(work[0])
            for i in range(len(work)):
                nxt = h_phase(work[i + 1]) if i + 1 < len(work) else None
                w2_phase(prev, first=(i == 0))
                prev = nxt
            for gi in range(2):
                g = 2 * th + gi
                self.ln_fm(acc[:, :, gi * 512:(gi + 1) * 512], bacc, self.V_LFG, self.V_LFB, g, final=final)
        if not final:
            self.dump(f"x2h{l}", self.XH[:], self.bX[3], [128, KC, S], BF16)
        self.phase_bufs.extend([b for (_, b) in self.wring4.items])
        self.wring = self.wring4


def host_inputs(inputs, b):
    m = {}
    m["x"] = np.ascontiguousarray(inputs["x"][b])
    for k in ("ln_in_g", "ln_in_b", "w_in", "a_q_norm", "a_k_norm", "c_q_norm", "c_kv_norm", "c_w_uq", "c_w_ukv",
              "d_conv_b", "d_ln_g", "d_ln_b", "w_branch", "w_out", "ln_mix_g", "ln_mix_b", "ffn_w1", "ffn_w3",
              "ffn_w2", "moe_router", "moe_w1", "moe_w3", "moe_w2", "ln_ffn_g", "ln_ffn_b"):
        m[k] = np.asarray(inputs[k], dtype=np.float32)
    m["d_conv_w"] = np.asarray(inputs["d_conv_w"], dtype=np.float32).reshape(DEPTH, 31, 256)
    cc = np.arange(GRID_W)
    dc = np.clip(cc[:, None] - cc[None, :] + 15, 0, 30)
    rpb = np.asarray(inputs["b_rpb"], dtype=np.float32)
    m["rpbT"] = np.ascontiguousarray(rpb[:, :, :, dc].reshape(DEPTH, 4, 15 * 64, 64))
    return m


_CONSTS = None


def consts():
    global _CONSTS
    if _CONSTS is None:
        _CONSTS = {"cst": make_consts(), "ropeA": make_rope(64, 2), "ropeC": make_rope(32, 1)}
    return _CONSTS


def kernel(**inputs):
    mk = MK()
    maps = []
    shared = None
    for b in range(8):
        m = host_inputs(inputs, b)
        m.update(consts())
        maps.append(m)
    res = run_bass_kernel_spmd(mk.nc, maps, core_ids=list(range(8)))
    out = np.stack([np.asarray(r["out"]) for r in res.results], axis=0)
    return out.astype(np.float32)
```
